# Optimizing a Trainium2 kernel written in Bass

```python
import jax, jax.numpy as jnp
from jax import lax
import numpy as np

D_MODEL = 1024
BATCH = 8
SEQ = 4096
DEPTH = 4

N_A = DEPTH // 2
N_B = DEPTH - N_A
PLE_DIM = 256
EPS = 1e-6
ROPE_THETA = 10000.0

A_HEADS = 4
A_DQK = 128
A_DV = 256
A_QK = A_HEADS * A_DQK
A_VW = A_HEADS * A_DV
A_CONV = 4
A_CHUNK = 64
A_COLS = 2 * A_QK + 3 * A_VW + 2 * A_HEADS

B_KV_HEADS = 4
B_HD = 128
DILATED_GROUPS = ((128, 1), (512, 4), (2048, 16))
B_GROUPS = len(DILATED_GROUPS)
B_QW = B_GROUPS * B_KV_HEADS * B_HD
B_VW = B_KV_HEADS * B_HD
B_COLS = B_QW + B_VW
Q_BLOCK = 128

kernel_name = "yoco_mlstm_dilated_swa_ple"


def rms_norm(x, g):
    xf = x.astype(jnp.float32)
    y = xf * lax.rsqrt(jnp.mean(xf * xf, axis=-1, keepdims=True) + EPS)
    return (y * g.astype(jnp.float32)).astype(x.dtype)


def rope_tables(seq, dim):
    inv = ROPE_THETA ** (-jnp.arange(0, dim, 2, dtype=jnp.float32) / dim)
    ang = jnp.arange(seq, dtype=jnp.float32)[:, None] * inv[None, :]
    return jnp.cos(ang), jnp.sin(ang)


def apply_rope(x, cos, sin):
    shape = (1, cos.shape[0]) + (1,) * (x.ndim - 3) + (cos.shape[1],)
    c = cos.reshape(shape)
    s = sin.reshape(shape)
    x1, x2 = jnp.split(x, 2, axis=-1)
    return jnp.concatenate([x1 * c - x2 * s, x2 * c + x1 * s], axis=-1)


def causal_depthwise_conv(x, w):
    K = w.shape[0]
    S = x.shape[1]
    xp = jnp.pad(x, ((0, 0), (K - 1, 0), (0, 0)))
    out = xp[:, 0:S] * w[0]
    for j in range(1, K):
        out = out + xp[:, j:j + S] * w[j]
    return out


def mlstm_chunkwise(q, k, v, i_pre, logf):
    Bsz, S, H, dk = q.shape
    dv = v.shape[-1]
    L = A_CHUNK
    nc = S // L

    def to_chunks(t):
        t = t.reshape((Bsz, nc, L, H) + t.shape[3:])
        return jnp.moveaxis(t, (1, 3), (0, 2))

    causal = jnp.tril(jnp.ones((L, L), dtype=bool))

    def step(carry, inp):
        C, n, m = carry
        qc, kc, vc, ic, fc = inp
        b = jnp.cumsum(fc, axis=-1)
        D = b[..., :, None] - b[..., None, :] + ic[..., None, :]
        D = jnp.where(causal, D, -jnp.inf)
        m_inter = b + m[..., None]
        m_s = jnp.maximum(m_inter, D.max(axis=-1))
        wts = jnp.exp(D - m_s[..., None]) * jnp.einsum('bhsd,bhjd->bhsj', qc, kc)
        inter = jnp.exp(m_inter - m_s)
        num = wts @ vc + inter[..., None] * jnp.einsum('bhsd,bhvd->bhsv', qc, C)
        den = wts.sum(axis=-1) + inter * jnp.einsum('bhsd,bhd->bhs', qc, n)
        h = num / jnp.maximum(jnp.abs(den), jnp.exp(-m_s))[..., None]
        g = b[..., -1:] - b + ic
        m_new = jnp.maximum(b[..., -1] + m, g.max(axis=-1))
        decay = jnp.exp(b[..., -1] + m - m_new)
        wj = jnp.exp(g - m_new[..., None])
        C = decay[..., None, None] * C + jnp.einsum('bhj,bhjv,bhjd->bhvd', wj, vc, kc)
        n = decay[..., None] * n + jnp.einsum('bhj,bhjd->bhd', wj, kc)
        return (C, n, m_new), h

    init = (jnp.zeros((Bsz, H, dv, dk), jnp.float32),
            jnp.zeros((Bsz, H, dk), jnp.float32),
            jnp.zeros((Bsz, H), jnp.float32))
    _, hs = lax.scan(step, init, (to_chunks(q), to_chunks(k), to_chunks(v),
                                  to_chunks(i_pre), to_chunks(logf)))
    return jnp.moveaxis(hs, (0, 2), (1, 3)).reshape(Bsz, S, H, dv)


def mlstm_layer(h, norm_g, w_in, conv_w, b_gate, hnorm_g, w_out):
    Bsz, S, _ = h.shape
    proj = rms_norm(h, norm_g) @ w_in
    qk, v, o, z, gates = jnp.split(
        proj, [2 * A_QK, 2 * A_QK + A_VW, 2 * A_QK + 2 * A_VW, 2 * A_QK + 3 * A_VW], axis=-1)
    qk = jax.nn.silu(causal_depthwise_conv(qk, conv_w)).astype(jnp.float32)
    q, k = jnp.split(qk, 2, axis=-1)
    q = q.reshape(Bsz, S, A_HEADS, A_DQK)
    k = k.reshape(Bsz, S, A_HEADS, A_DQK) * (A_DQK ** -0.5)
    v = v.astype(jnp.float32).reshape(Bsz, S, A_HEADS, A_DV)
    gates = gates.astype(jnp.float32) + b_gate.astype(jnp.float32)
    i_pre, f_pre = jnp.split(gates, 2, axis=-1)
    ht = mlstm_chunkwise(q, k, v, i_pre, jax.nn.log_sigmoid(f_pre))
    ht = jax.nn.sigmoid(o.astype(jnp.float32)).reshape(Bsz, S, A_HEADS, A_DV) * ht
    ht = rms_norm(ht, hnorm_g.reshape(A_HEADS, A_DV))
    y = ht.reshape(Bsz, S, A_VW) * jax.nn.silu(z.astype(jnp.float32))
    return y.astype(h.dtype) @ w_out


def dilated_window_attention(q, k, v, window, dilation):
    Bsz, S, H, hd = q.shape
    r = dilation
    n_back = window // dilation
    s_sub = S // r
    nb = -(-s_sub // Q_BLOCK)
    P = nb * Q_BLOCK

    def to_blocks(t):
        t = t.reshape(Bsz, s_sub, r, H, hd).transpose(0, 2, 3, 1, 4)
        t = jnp.pad(t, ((0, 0), (0, 0), (0, 0), (0, P - s_sub), (0, 0)))
        return t.reshape(Bsz, r, H, nb, Q_BLOCK, hd)

    def with_prev(t):
        prev = jnp.pad(t, ((0, 0), (0, 0), (0, 0), (1, 0), (0, 0), (0, 0)))[:, :, :, :-1]
        return jnp.concatenate([prev, t], axis=4)

    qb = to_blocks(q)
    kw = with_prev(to_blocks(k))
    vw = with_prev(to_blocks(v))
    s = jnp.einsum('brhnqd,brhnkd->brhnqk', qb, kw) * (hd ** -0.5)
    blk = jnp.arange(nb)[:, None, None]
    qi = jnp.arange(Q_BLOCK)[None, :, None]
    kj = jnp.arange(2 * Q_BLOCK)[None, None, :]
    dist = Q_BLOCK + qi - kj
    kpos = (blk - 1) * Q_BLOCK + kj
    mask = (dist >= 0) & (dist <= n_back) & (kpos >= 0)
    s = jnp.where(mask, s, -jnp.inf)
    mx = s.max(axis=-1, keepdims=True)
    e = jnp.exp(s - mx)
    den = e.sum(axis=-1)
    o = jnp.einsum('brhnqk,brhnkd->brhnqd', e, vw) / den[..., None]
    lse = mx[..., 0] + jnp.log(den)
    o = o.reshape(Bsz, r, H, P, hd)[:, :, :, :s_sub].transpose(0, 3, 1, 2, 4).reshape(Bsz, S, H, hd)
    lse = lse.reshape(Bsz, r, H, P)[..., :s_sub].transpose(0, 3, 1, 2).reshape(Bsz, S, H)
    return o, lse


def shared_kv(h, norm_g, w_kv, knorm_g, cos, sin):
    Bsz, S, _ = h.shape
    kv = (rms_norm(h, norm_g) @ w_kv).astype(jnp.float32)
    k, v = jnp.split(kv, 2, axis=-1)
    k = k.reshape(Bsz, S, B_KV_HEADS, B_HD)
    v = v.reshape(Bsz, S, B_KV_HEADS, B_HD)
    k = apply_rope(rms_norm(k, knorm_g), cos, sin)
    return k, v


def dilated_layer(h, k, v, norm_g, w_in, qnorm_g, w_out, cos, sin):
    Bsz, S, _ = h.shape
    proj = rms_norm(h, norm_g) @ w_in
    q, z = jnp.split(proj, [B_QW], axis=-1)
    q = q.astype(jnp.float32).reshape(Bsz, S, B_GROUPS, B_KV_HEADS, B_HD)
    q = apply_rope(rms_norm(q, qnorm_g), cos, sin)
    outs, lses = [], []
    for g, (window, dilation) in enumerate(DILATED_GROUPS):
        o_g, l_g = dilated_window_attention(q[:, :, g], k, v, window, dilation)
        outs.append(o_g)
        lses.append(l_g)
    wts = jax.nn.softmax(jnp.stack(lses, axis=0), axis=0)
    o = jnp.einsum('gbsh,gbshd->bshd', wts, jnp.stack(outs, axis=0))
    y = o.reshape(Bsz, S, B_VW) * jax.nn.silu(z.astype(jnp.float32))
    return y.astype(h.dtype) @ w_out


def ple_add(h, p_l, norm_g, w_gate, w_proj):
    gate = jax.nn.sigmoid((rms_norm(h, norm_g) @ w_gate).astype(jnp.float32))
    return h + ((p_l @ w_proj).astype(jnp.float32) * gate).astype(h.dtype)


def setup_inputs(seed: int = 0) -> dict:
    key = jax.random.key(seed)
    ks = jax.random.split(key, 24)
    nrm = jax.random.normal
    f32 = jnp.float32
    x = nrm(ks[0], (BATCH, SEQ, D_MODEL), f32)
    p = nrm(ks[1], (DEPTH, BATCH, SEQ, PLE_DIM), f32)
    norm_a = 1.0 + 0.05 * nrm(ks[2], (N_A, D_MODEL), f32)
    w_in_a = nrm(ks[3], (N_A, D_MODEL, A_COLS), f32) * D_MODEL ** -0.5
    conv_a = nrm(ks[4], (N_A, A_CONV, 2 * A_QK), f32) * A_CONV ** -0.5
    b_i = 0.1 * nrm(ks[5], (N_A, A_HEADS), f32)
    b_f = jnp.linspace(3.0, 6.0, A_HEADS, dtype=f32)[None, :] + 0.1 * nrm(ks[6], (N_A, A_HEADS), f32)
    b_gate_a = jnp.concatenate([b_i, b_f], axis=-1)
    hnorm_a = 1.0 + 0.05 * nrm(ks[7], (N_A, A_VW), f32)
    w_out_a = nrm(ks[8], (N_A, A_VW, D_MODEL), f32) * A_VW ** -0.5
    norm_kv = 1.0 + 0.05 * nrm(ks[9], (D_MODEL,), f32)
    w_kv = nrm(ks[10], (D_MODEL, 2 * B_VW), f32) * D_MODEL ** -0.5
    knorm = 1.0 + 0.05 * nrm(ks[11], (B_HD,), f32)
    norm_b = 1.0 + 0.05 * nrm(ks[12], (N_B, D_MODEL), f32)
    w_in_b = nrm(ks[13], (N_B, D_MODEL, B_COLS), f32) * D_MODEL ** -0.5
    qnorm_b = 1.0 + 0.05 * nrm(ks[14], (N_B, B_HD), f32)
    w_out_b = nrm(ks[15], (N_B, B_VW, D_MODEL), f32) * B_VW ** -0.5
    ple_norm = 1.0 + 0.05 * nrm(ks[16], (DEPTH, D_MODEL), f32)
    w_ple_gate = nrm(ks[17], (DEPTH, D_MODEL, D_MODEL), f32) * D_MODEL ** -0.5
    w_ple = nrm(ks[18], (DEPTH, PLE_DIM, D_MODEL), f32) * PLE_DIM ** -0.5
    return {"x": x, "p": p, "norm_a": norm_a, "w_in_a": w_in_a, "conv_a": conv_a,
            "b_gate_a": b_gate_a, "hnorm_a": hnorm_a, "w_out_a": w_out_a,
            "norm_kv": norm_kv, "w_kv": w_kv, "knorm": knorm,
            "norm_b": norm_b, "w_in_b": w_in_b, "qnorm_b": qnorm_b, "w_out_b": w_out_b,
            "ple_norm": ple_norm, "w_ple_gate": w_ple_gate, "w_ple": w_ple}


def reference(x, p, norm_a, w_in_a, conv_a, b_gate_a, hnorm_a, w_out_a,
              norm_kv, w_kv, knorm, norm_b, w_in_b, qnorm_b, w_out_b,
              ple_norm, w_ple_gate, w_ple):
    S = x.shape[1]
    cos, sin = rope_tables(S, B_HD)
    h = x
    k_sh = None
    v_sh = None
    for layer in range(DEPTH):
        if layer < N_A:
            h = h + mlstm_layer(h, norm_a[layer], w_in_a[layer], conv_a[layer],
                                b_gate_a[layer], hnorm_a[layer], w_out_a[layer])
        else:
            if layer == N_A:
                k_sh, v_sh = shared_kv(h, norm_kv, w_kv, knorm, cos, sin)
            j = layer - N_A
            h = h + dilated_layer(h, k_sh, v_sh, norm_b[j], w_in_b[j], qnorm_b[j],
                                  w_out_b[j], cos, sin)
        h = ple_add(h, p[layer], ple_norm[layer], w_ple_gate[layer], w_ple[layer])
    return h
```

```python
import contextlib
import math
import numpy as np
import concourse.bass as bass
import concourse.mybir as mybir
from concourse.bass_utils import run_bass_kernel_spmd

F32 = mybir.dt.float32
BF16 = mybir.dt.bfloat16
AF = mybir.ActivationFunctionType
ALU = mybir.AluOpType

S_LEN = 4096
NT = S_LEN // 128
D = 1024
EPS = 1e-6
ENGS = ("pe", "act", "dve", "pool", "sp")


class Op:
    __slots__ = ("eng", "fn", "deps", "need_ms", "ms", "dom", "is_dma", "chan",
                 "ndma", "dval", "clock", "waits", "epoch")


class Sched:
    def __init__(self, nc, same_engine_sync=True):
        self.nc = nc
        self.ops = {e: [] for e in ENGS}
        self.order = []
        self.state = {}
        self.epoch = 0
        self.same_engine_sync = same_engine_sync
        self.chan_count = {}
        self.chans = []
        self.last = {}
        self.pending = {e: [] for e in ENGS}

    def new_epoch(self):
        self.epoch += 1

    def barrier(self):
        deps = list(self.last.values())
        for e in ENGS:
            self.pending[e] = list(deps)

    def _deps_for(self, eng, reads, writes):
        deps = []
        for k in reads:
            st = self.state.get(k)
            if st is not None and st[0] is not None:
                deps.append(st[0])
        for k in writes:
            st = self.state.get(k)
            if st is not None:
                if st[0] is not None:
                    deps.append(st[0])
                lastr = {}
                for r_ in st[1]:
                    if r_.is_dma:
                        deps.append(r_)
                    else:
                        lastr[(r_.eng, r_.epoch)] = r_
                deps.extend(lastr.values())
        if self.pending[eng]:
            deps.extend(self.pending[eng])
            self.pending[eng] = []
        return deps

    def _commit(self, op, reads, writes):
        for k in reads:
            st = self.state.setdefault(k, [None, []])
            st[1].append(op)
        for k in writes:
            self.state[k] = [op, []]

    def op(self, eng, fn, reads=(), writes=()):
        o = Op()
        o.eng = eng; o.fn = fn; o.need_ms = False; o.ms = None
        o.is_dma = False; o.epoch = self.epoch
        o.deps = self._deps_for(eng, reads, writes)
        self._commit(o, reads, writes)
        self.ops[eng].append(o)
        self.order.append(o)
        self.last[eng] = o
        return o

    def dma(self, eng, chan, fns, reads=(), writes=()):
        o = Op()
        o.eng = eng; o.fn = fns; o.need_ms = False; o.ms = None
        o.is_dma = True; o.chan = chan; o.ndma = len(fns); o.epoch = self.epoch
        if chan not in self.chan_count:
            self.chan_count[chan] = 0
            self.chans.append(chan)
        self.chan_count[chan] += len(fns)
        o.dval = 16 * self.chan_count[chan]
        o.deps = self._deps_for(eng, reads, writes)
        self._commit(o, reads, writes)
        self.ops[eng].append(o)
        self.order.append(o)
        self.last[("dma", chan)] = o
        return o

    def _skip(self, d, o):
        return (not d.is_dma) and d.eng == o.eng and (not o.is_dma) and \
            (o.eng == "pe" or not self.same_engine_sync)

    def finalize_and_emit(self, final_waits=()):
        nc = self.nc
        for o in self.order:
            for d in o.deps:
                if d.is_dma or self._skip(d, o):
                    continue
                d.need_ms = True
        cnt = {}
        for o in self.order:
            if o.is_dma:
                o.dom = ("dma", o.chan)
                continue
            o.dom = (o.eng, o.epoch)
            if o.need_ms:
                cnt[o.dom] = cnt.get(o.dom, 0) + 1
                o.ms = cnt[o.dom]
        eclock = {e: {} for e in ENGS}
        nwaits = 0
        for o in self.order:
            ck = eclock[o.eng]
            waits = {}
            for d in o.deps:
                if d.is_dma:
                    dom, val = d.dom, d.dval
                else:
                    if self._skip(d, o):
                        continue
                    dom, val = d.dom, d.ms
                if ck.get(dom, 0) >= val:
                    continue
                if waits.get(dom, 0) < val:
                    waits[dom] = val
            for d in o.deps:
                if d.is_dma:
                    dom, val = d.dom, d.dval
                else:
                    dom, val = d.dom, d.ms
                if dom in waits and waits[dom] == val and d.clock is not None:
                    for k, v in d.clock.items():
                        if ck.get(k, 0) < v:
                            ck[k] = v
            for dom, val in waits.items():
                if ck.get(dom, 0) < val:
                    ck[dom] = val
            o.waits = list(waits.items())
            nwaits += len(o.waits)
            if o.is_dma:
                c = dict(ck); c[o.dom] = o.dval
                o.clock = c
            elif o.ms is not None:
                c = dict(ck); c[o.dom] = o.ms
                if o.eng == "pe" or not self.same_engine_sync:
                    ck[o.dom] = o.ms
                o.clock = c
            else:
                o.clock = None
        self.nwaits = nwaits
        stack = contextlib.ExitStack()
        sems = {}
        doms = list(cnt.keys()) + [("dma", c) for c in self.chans]
        for i, dom in enumerate(doms):
            sems[dom] = stack.enter_context(nc.semaphore("s%d" % i))
        fw = [(sems[d.dom], d.dval) for d in final_waits]
        ops = self.ops
        with stack:
            with nc.Block() as block:
                def emit(engname, engobj, last=False):
                    for o in ops[engname]:
                        for dom, val in o.waits:
                            engobj.wait_ge(sems[dom], val)
                        if o.is_dma:
                            for f in o.fn:
                                f(engobj).then_inc(sems[o.dom], 16)
                        else:
                            ins = o.fn(engobj)
                            if o.ms is not None:
                                ins.then_inc(sems[o.dom], 1)
                    if last:
                        for s, v in fw:
                            engobj.wait_ge(s, v)

                @block.tensor
                def _(e):
                    emit("pe", e)

                @block.scalar
                def _(e):
                    emit("act", e)

                @block.vector
                def _(e):
                    emit("dve", e)

                @block.gpsimd
                def _(e):
                    emit("pool", e)

                @block.sync
                def _(e):
                    emit("sp", e, last=True)


def _pk(v):
    return np.ascontiguousarray(np.asarray(v, np.float32).reshape(-1, 128).T)


CV = {}
_c = 0
for _l in range(2):
    CV[("norm_a", _l)] = _c; _c += 8
    CV[("hnorm_a", _l)] = _c; _c += 8
    CV[("conv_a", _l)] = _c; _c += 32
CV["norm_kv"] = _c; _c += 8
for _l in range(2):
    CV[("norm_b", _l)] = _c; _c += 8
for _l in range(4):
    CV[("ple_norm", _l)] = _c; _c += 8
NCV = _c
RW = {}
_c = 0
for _l in range(2):
    RW[("b_gate", _l)] = _c; _c += 8
RW["knorm"] = _c; _c += 128
for _l in range(2):
    RW[("qnorm", _l)] = _c; _c += 128
NRW = _c
LNSK = math.log(128.0 ** -0.5)


def build_program(n_layers=4, ntiles=NT):
    nc = bass.Bass("TRN2", target_bir_lowering=False)
    S_ = ntiles * 128

    def din(name, shape):
        return nc.dram_tensor(name, shape, F32, kind="ExternalInput").ap()

    x_d = din("x", [S_LEN, D])
    p_d = din("p", [4, S_LEN, 256])
    w_in_a_d = din("w_in_a", [2, 128, 8, 4104])
    w_out_a_d = din("w_out_a", [2, 128, 8, 1024])
    w_kv_d = din("w_kv", [128, 8, 1024])
    w_in_b_d = din("w_in_b", [2, 128, 8, 2048])
    w_out_b_d = din("w_out_b", [2, 128, 4, 1024])
    w_pg_d = din("w_ple_gate", [4, 128, 8, 1024])
    w_pp_d = din("w_ple", [4, 128, 2, 1024])
    cvec_d = din("cvec", [128, NCV])
    rows_d = din("rows", [128, NRW])
    cos_d = din("cos", [128, NT, 64])
    sin_d = din("sin", [128, NT, 64])
    cst_d = din("cst", [128, 4, 128])
    out_d = nc.dram_tensor("out", [S_LEN, D], F32, kind="ExternalOutput").ap()
    hA_d = nc.dram_tensor("hA", [S_LEN, D], F32).ap()
    hB_d = nc.dram_tensor("hB", [S_LEN, D], F32).ap()
    qT_d = nc.dram_tensor("qT", [4, 128, 3, S_LEN], BF16).ap()

    S = Sched(nc)
    OP = S.op
    gs = contextlib.ExitStack()

    uid = [0]

    def sb(es, name, shape, dt):
        uid[0] += 1
        return es.enter_context(nc.sbuf_tensor("%s_u%d" % (name, uid[0]), shape, dt))

    cnt = {"b": 0, "f": 0, "n": 0, "stg": 0, "cv": 0}

    with gs:
        cstf = sb(gs, "cstf", [128, 4, 128], F32)
        identb = sb(gs, "identb", [128, 128], BF16)
        triub = sb(gs, "triub", [128, 128], BF16)
        mask2 = sb(gs, "mask2", [128, 4, 128], BF16)
        onesb = sb(gs, "onesb", [128, 128], BF16)
        cvec = sb(gs, "cvec", [128, NCV], F32)
        rows = sb(gs, "rows", [128, NRW], F32)
        PSB = [gs.enter_context(nc.psum_tensor("psb%d" % i, [128, 1024], BF16)) for i in range(2)]
        PSF = [gs.enter_context(nc.psum_tensor("psf%d" % i, [128, 512], F32)) for i in range(6)]
        triuf = cstf[:, 1, :]
        onesf = cstf[:, 3, :]

        def nextb():
            i = cnt["b"] % 2; cnt["b"] += 1
            return PSB[i], ("pb", i)

        def nextf():
            i = cnt["f"] % 6; cnt["f"] += 1
            return PSF[i], ("pf", i)

        S.dma("sp", "cst", [lambda e: e.dma_start(out=cstf[:], in_=cst_d),
                            lambda e: e.dma_start(out=cvec[:], in_=cvec_d),
                            lambda e: e.dma_start(out=rows[:], in_=rows_d)],
              writes=["cstf", "cvec", "rows"])
        OP("dve", lambda e: e.tensor_copy(out=identb[:], in_=cstf[:, 0, :]), ["cstf"], ["identb"])
        OP("dve", lambda e: e.tensor_copy(out=triub[:], in_=cstf[:, 1, :]), ["cstf"], ["triub"])
        OP("dve", lambda e: e.tensor_copy(out=onesb[:], in_=cstf[:, 3, :]), ["cstf"], ["onesb"])
        for i in range(4):
            OP("dve", lambda e, i=i: e.tensor_copy(out=mask2[:, i, :], in_=cstf[:, 1 + (i % 2), :]),
               ["cstf"], ["mask2"])

        def mm(out, lhsT, rhs, start, stop, r, w):
            OP("pe", lambda e: e.matmul(out=out, lhsT=lhsT, rhs=rhs, start=start, stop=stop), r, w)

        def sigmoid_inplace(dst, src, rkeys, wkey):
            OP("act", lambda e: e.activation(out=dst, in_=src, func=AF.Exp, scale=-1.0), rkeys, [wkey])
            OP("act", lambda e: e.activation(out=dst, in_=dst, func=AF.Ln, bias=1.0), [wkey], [wkey])
            OP("act", lambda e: e.activation(out=dst, in_=dst, func=AF.Exp, scale=-1.0), [wkey], [wkey])

        def load_weights(specs, nstg=4):
            with contextlib.ExitStack() as ws:
                stg = [sb(ws, "stgL%d" % i, [128, 4104], F32) for i in range(nstg)]
                for dst, src, K, N, gcol, wkey in specs:
                    kk = max(1, 4104 // N)
                    for k0 in range(0, K, kk):
                        k1 = min(K, k0 + kk)
                        si = cnt["stg"] % nstg; cnt["stg"] += 1
                        sv = stg[si][:, 0:(k1 - k0) * N].rearrange("p (k n) -> p k n", k=k1 - k0)
                        S.dma("sp", ("stg", si),
                              [lambda e, sv=sv, src=src, k0=k0, k1=k1: e.dma_start(out=sv, in_=src[:, k0:k1, :])],
                              writes=[("stg", si)])
                        for k in range(k0, k1):
                            eng = ("act", "dve")[cnt["cv"] % 2]; cnt["cv"] += 1
                            o_ap = dst[:, k, :]
                            i_ap = sv[:, k - k0, :]
                            if gcol is None:
                                if eng == "act":
                                    fn = lambda e, o_ap=o_ap, i_ap=i_ap: e.activation(out=o_ap, in_=i_ap, func=AF.Copy)
                                else:
                                    fn = lambda e, o_ap=o_ap, i_ap=i_ap: e.tensor_copy(out=o_ap, in_=i_ap)
                            else:
                                g_ap = cvec[:, gcol + k:gcol + k + 1]
                                if eng == "act":
                                    fn = lambda e, o_ap=o_ap, i_ap=i_ap, g_ap=g_ap: e.activation(
                                        out=o_ap, in_=i_ap, func=AF.Copy, scale=g_ap)
                                else:
                                    fn = lambda e, o_ap=o_ap, i_ap=i_ap, g_ap=g_ap: e.tensor_scalar(
                                        out=o_ap, in0=i_ap, scalar1=g_ap, scalar2=None, op0=ALU.mult)
                            OP(eng, fn, [("stg", si), "cvec"], [(wkey, eng)])
                S.barrier()

        def wk(name):
            return [(name, "act"), (name, "dve")]

        def norm_T(bufs, src, skey, slot=None, dim=D):
            if slot is None:
                i = cnt["n"] % 2; cnt["n"] += 1
            else:
                i = slot
            junk, ss, rstd, xs, xsT = bufs["junk"], bufs["ss"], bufs["rstd"], bufs["xs"], bufs["xsT"]
            OP("act", lambda e: e.activation(out=junk[:], in_=src, func=AF.Square, accum_out=ss[:, i:i + 1]),
               [skey], ["junk", ("ss", i)])
            OP("act", lambda e: e.activation(out=rstd[:, i:i + 1], in_=ss[:, i:i + 1], func=AF.Ln,
                                             scale=1.0 / dim, bias=EPS), [("ss", i)], [("rstd", i)])
            OP("act", lambda e: e.activation(out=rstd[:, i:i + 1], in_=rstd[:, i:i + 1], func=AF.Exp, scale=-0.5),
               [("rstd", i)], [("rstd", i)])
            OP("dve", lambda e: e.tensor_scalar(out=xs[i][:], in0=src, scalar1=rstd[:, i:i + 1], scalar2=None,
                                                op0=ALU.mult), [skey, ("rstd", i)], [("xs", i)])
            pb, pbk = nextb()
            for k in range(8):
                OP("pe", lambda e, k=k: e.transpose(out=pb[:, k * 128:(k + 1) * 128],
                                                    in_=xs[i][:, k * 128:(k + 1) * 128], identity=identb[:]),
                   [("xs", i), "identb"], [pbk])
            OP("act", lambda e: e.activation(out=xsT[i][:].rearrange("p k t -> p (k t)"), in_=pb[:], func=AF.Copy),
               [pbk], [("xsT", i)])
            return xsT[i], ("xsT", i)

        def alloc_ple_w(es):
            b = {}
            b["Wg"] = sb(es, "Wg", [128, 8, 1024], BF16)
            b["Wp"] = sb(es, "Wp", [128, 2, 1024], BF16)
            return b

        def alloc_common(es, b):
            b["ht"] = [sb(es, "ht%d" % i, [128, D], F32) for i in range(3)]
            b["junk"] = sb(es, "junk", [128, D], BF16)
            b["ss"] = sb(es, "ss", [128, 4], F32)
            b["rstd"] = sb(es, "rstd", [128, 4], F32)
            b["xs"] = [sb(es, "xs%d" % i, [128, D], BF16) for i in range(3)]
            b["xsT"] = [sb(es, "xsT%d" % i, [128, 8, 128], BF16) for i in range(3)]
            b["pt"] = [sb(es, "pt%d" % i, [128, 256], F32) for i in range(2)]
            b["pb16"] = sb(es, "pb16", [128, 256], BF16)
            b["pT"] = sb(es, "pT", [128, 2, 128], BF16)
            b["TP"] = sb(es, "TP", [128, D], F32)
            return b

        def load_h(bufs, hsrc, t):
            hs = t % 3
            S.dma("sp", ("ht", hs),
                  [lambda e: e.dma_start(out=bufs["ht"][hs][:], in_=hsrc[t * 128:(t + 1) * 128, :])],
                  writes=[("ht", hs)])

        def load_p(bufs, l, t):
            s2 = t % 2
            S.dma("sp", ("pt", s2),
                  [lambda e: e.dma_start(out=bufs["pt"][s2][:], in_=p_d[l, t * 128:(t + 1) * 128, :])],
                  writes=[("pt", s2)])

        def load_tile(bufs, l, hsrc, t):
            hs = t % 3
            S.dma("sp", ("ht", hs),
                  [lambda e: e.dma_start(out=bufs["ht"][hs][:], in_=hsrc[t * 128:(t + 1) * 128, :])],
                  writes=[("ht", hs)])
            s2 = t % 2
            S.dma("sp", ("pt", s2),
                  [lambda e: e.dma_start(out=bufs["pt"][s2][:], in_=p_d[l, t * 128:(t + 1) * 128, :])],
                  writes=[("pt", s2)])

        def ple_and_store(bufs, l, t, hdst):
            hs = t % 3
            X = bufs["ht"][hs]
            hk = ("ht", hs)
            s2 = t % 2
            xT2, xT2k = norm_T(bufs, X[:], hk, slot=2)
            pb16, pT, TP = bufs["pb16"], bufs["pT"], bufs["TP"]
            OP("dve", lambda e: e.tensor_copy(out=pb16[:], in_=bufs["pt"][s2][:]), [("pt", s2)], ["pb16"])
            pb, pbk = nextb()
            for k in range(2):
                OP("pe", lambda e, k=k: e.transpose(out=pb[:, k * 128:(k + 1) * 128],
                                                    in_=pb16[:, k * 128:(k + 1) * 128], identity=identb[:]),
                   ["pb16", "identb"], [pbk])
            OP("dve", lambda e: e.tensor_copy(out=pT[:].rearrange("p k t -> p (k t)"), in_=pb[:, 0:256]),
               [pbk], ["pT"])
            Wg, Wp = bufs["Wg"], bufs["Wp"]
            for nb in range(2):
                sl = slice(nb * 512, (nb + 1) * 512)
                pg, pgk = nextf()
                for k in range(8):
                    mm(pg[:], xT2[:, k, :], Wg[:, k, sl], k == 0, k == 7, [xT2k] + wk("Wg"), [pgk])
                pp, ppk = nextf()
                for k in range(2):
                    mm(pp[:], pT[:, k, :], Wp[:, k, sl], k == 0, k == 1, ["pT"] + wk("Wp"), [ppk])
                tk = ("TP", nb)
                sigmoid_inplace(TP[:, sl], pg[:], [pgk], tk)
                OP("dve", lambda e, sl=sl, pp=pp: e.tensor_tensor(out=TP[:, sl], in0=pp[:], in1=TP[:, sl],
                                                                 op=ALU.mult), [ppk, tk], [tk])
                OP("dve", lambda e, sl=sl: e.tensor_tensor(out=X[:, sl], in0=X[:, sl], in1=TP[:, sl], op=ALU.add),
                   [hk, tk], [hk])
            return S.dma("pool", ("st", hs),
                         [lambda e: e.dma_start(out=hdst[t * 128:(t + 1) * 128, :], in_=X[:])], reads=[hk])

        def layer_A(l, hsrc, hdst):
            S.new_epoch()
            S.barrier()
            es = contextlib.ExitStack()
            with es:
                bufs = alloc_ple_w(es)
                Win = sb(es, "Win", [128, 8, 4104], BF16)
                Wout = sb(es, "Wout", [128, 8, 1024], BF16)
                load_weights([(Win, w_in_a_d[l], 8, 4104, CV[("norm_a", l)], "Win"),
                              (Wout, w_out_a_d[l], 8, 1024, CV[("hnorm_a", l)], "Wout"),
                              (bufs["Wg"], w_pg_d[l], 8, 1024, CV[("ple_norm", l)], "Wg"),
                              (bufs["Wp"], w_pp_d[l], 2, 1024, None, "Wp")])
                alloc_common(es, bufs)
                xq = sb(es, "xq", [128, 8, 131], F32)
                T0 = sb(es, "T0", [128, D], F32)
                T1 = sb(es, "T1", [128, D], F32)
                T2 = [sb(es, "T2_%d" % i, [128, D], BF16) for i in range(2)]
                T3 = [sb(es, "T3_%d" % i, [128, D], BF16) for i in range(2)]
                T3sb = [sb(es, "T3s%d" % i, [128, 512], F32) for i in range(2)]
                qkT = [sb(es, "qkT%d" % i, [128, 8, 128], BF16) for i in range(2)]
                Ktok = [sb(es, "Ktok%d" % i, [128, 512], BF16) for i in range(2)]
                uV = [sb(es, "uV%d" % i, [128, 4, 258], BF16) for i in range(2)]
                PT = sb(es, "PT", [128, 4, 128], BF16)
                ho = sb(es, "ho", [128, D], F32)
                yb = sb(es, "yb", [128, D], BF16)
                yT = sb(es, "yT", [128, 8, 128], BF16)
                Dst = sb(es, "Dst", [128, 4, 257], F32)
                Cb = sb(es, "Cb", [128, 4, 258], BF16)
                sm = sb(es, "sm", [128, 96], F32)
                g8 = sm[:, 0:8]; l4 = sm[:, 8:12]; iB = sm[:, 16:20]
                u4 = sm[:, 20:24]; t1 = sm[:, 24:28]; r4 = sm[:, 28:32]; ssh = sm[:, 32:36]
                rsh = sm[:, 36:40]
                E3c = [40, 44, 48]
                eBc = [52, 56]
                T0v = T0[:].rearrange("p (c t) -> p c t", c=8)
                T1v = T1[:].rearrange("p (c t) -> p c t", c=8)
                OP("pool", lambda e: e.memset(xq[:, :, 0:3], 0.0), [], [("xq", 0), ("xq", 1)])
                cb = CV[("conv_a", l)]
                bg = RW[("b_gate", l)]
                ctx = {}

                def s0(t):
                    hs = t % 3
                    X = bufs["ht"][hs]
                    ctx[t] = norm_T(bufs, X[:], ("ht", hs), slot=t % 2)

                def s1(t):
                    i2 = t % 2
                    xT, xTk = ctx.pop(t)
                    eBo = eBc[i2]
                    Eo = E3c[t % 3]
                    eB = sm[:, eBo:eBo + 4]
                    Ecur = sm[:, Eo:Eo + 4]
                    pfg, pfgk = nextf()
                    for k in range(8):
                        mm(pfg[:, 0:8], xT[:, k, :], Win[:, k, 4096:4104], k == 0, k == 7, [xTk] + wk("Win"), [pfgk])
                    OP("dve", lambda e: e.tensor_tensor(out=g8, in0=pfg[:, 0:8], in1=rows[:, bg:bg + 8],
                                                        op=ALU.add), [pfgk, "rows"], ["g8"])
                    OP("act", lambda e: e.activation(out=l4, in_=sm[:, 4:8], func=AF.Exp, scale=-1.0), ["g8"], ["l4"])
                    OP("act", lambda e: e.activation(out=l4, in_=l4, func=AF.Ln, bias=1.0), ["l4"], ["l4"])
                    mm(pfg[:, 8:12], triuf, l4, True, True, ["cstf", "l4"], [pfgk])
                    mm(pfg[:, 12:16], onesf, l4, True, True, ["cstf", "l4"], [pfgk])
                    OP("act", lambda e: e.activation(out=eB, in_=pfg[:, 8:12], func=AF.Exp, scale=-1.0),
                       [pfgk], [("eB", i2)])
                    OP("dve", lambda e: e.tensor_tensor(out=iB, in0=sm[:, 0:4], in1=pfg[:, 8:12], op=ALU.add),
                       [pfgk, "g8"], ["iB"])
                    OP("act", lambda e: e.activation(out=u4, in_=iB, func=AF.Exp, bias=LNSK), ["iB"], ["u4"])
                    OP("act", lambda e: e.activation(out=Ecur, in_=pfg[:, 12:16], func=AF.Exp, scale=-1.0),
                       [pfgk], [("E", t % 3)])
                    if t > 0:
                        OP("dve", lambda e: e.tensor_copy(out=xq[:, :, 0:3], in_=xq[:, :, 128:131]),
                           [("xq", 0), ("xq", 1)], [("xq", 0), ("xq", 1)])
                    for half in range(2):
                        pf, pfk = nextf()
                        for c4 in range(4):
                            c = half * 4 + c4
                            for k in range(8):
                                mm(pf[:, c4 * 128:(c4 + 1) * 128], Win[:, k, c * 128:(c + 1) * 128], xT[:, k, :],
                                   k == 0, k == 7, [xTk] + wk("Win"), [pfk])
                        OP("act", lambda e, pf=pf, half=half: e.activation(
                            out=xq[:, half * 4:half * 4 + 4, 3:131],
                            in_=pf[:].rearrange("p (c t) -> p c t", c=4), func=AF.Copy), [pfk], [("xq", half)])

                    def wc(j):
                        return cvec[:, cb + j * 8:cb + j * 8 + 8].unsqueeze(2).to_broadcast([128, 8, 128])
                    xqk = [("xq", 0), ("xq", 1)]
                    OP("dve", lambda e: e.tensor_tensor(out=T0v, in0=xq[:, :, 3:131], in1=wc(3), op=ALU.mult),
                       xqk + ["cvec"], ["T0"])
                    for j in (2, 1, 0):
                        OP("dve", lambda e, j=j: e.tensor_tensor(out=T1v, in0=xq[:, :, j:j + 128], in1=wc(j),
                                                                op=ALU.mult), xqk + ["cvec"], ["T1"])
                        OP("dve", lambda e: e.tensor_tensor(out=T0[:], in0=T0[:], in1=T1[:], op=ALU.add),
                           ["T0", "T1"], ["T0"])
                    sigmoid_inplace(T1[:], T0[:], ["T0"], "T1")
                    qk = qkT[i2]
                    qkk = ("qkT", i2)
                    OP("dve", lambda e: e.tensor_tensor(out=qk[:].rearrange("p c t -> p (c t)"), in0=T0[:],
                                                        in1=T1[:], op=ALU.mult), ["T0", "T1"], [qkk])
                    pb, pbk = nextb()
                    for h in range(4):
                        OP("pe", lambda e, h=h: e.transpose(out=pb[:, h * 128:(h + 1) * 128],
                                                            in_=qk[:, 4 + h, :], identity=identb[:]),
                           [qkk, "identb"], [pbk])
                    Kt = Ktok[i2]
                    Ktk = ("Ktok", i2)
                    OP("dve", lambda e: e.tensor_copy(out=Kt[:], in_=pb[:, 0:512]), [pbk], [Ktk])
                    uVc = uV[i2]
                    uVk = ("uV", i2)
                    T2c = T2[i2]
                    T3c = T3[i2]
                    for nb in range(6):
                        pf, pfk = nextf()
                        for k in range(8):
                            mm(pf[:], xT[:, k, :], Win[:, k, 1024 + nb * 512:1024 + (nb + 1) * 512],
                               k == 0, k == 7, [xTk] + wk("Win"), [pfk])
                        if nb < 2:
                            for hh in range(2):
                                h = 2 * nb + hh
                                OP("act", lambda e, h=h, hh=hh, pf=pf: e.activation(
                                    out=uVc[:, h, 0:256], in_=pf[:, hh * 256:(hh + 1) * 256], func=AF.Copy,
                                    scale=sm[:, 20 + h:21 + h]), [pfk, "u4"], [uVk])
                        elif nb < 4:
                            sl = slice((nb - 2) * 512, (nb - 1) * 512)
                            T3s = T3sb[nb % 2]; tsk = ("T3s", nb % 2)
                            sigmoid_inplace(T3s[:], pf[:], [pfk], tsk)
                            OP("dve", lambda e, sl=sl, T3s=T3s: e.tensor_copy(out=T2c[:, sl], in_=T3s[:]),
                               [tsk], [("T2", i2, nb - 2)])
                        else:
                            sl = slice((nb - 4) * 512, (nb - 3) * 512)
                            T3s = T3sb[nb % 2]; tsk = ("T3s", nb % 2)
                            sigmoid_inplace(T3s[:], pf[:], [pfk], tsk)
                            OP("dve", lambda e, sl=sl, pf=pf, T3s=T3s: e.tensor_tensor(out=T3c[:, sl], in0=pf[:],
                                                                                      in1=T3s[:], op=ALU.mult),
                               [pfk, tsk], [("T3", i2, nb - 4)])
                    OP("dve", lambda e: e.tensor_copy(out=uVc[:, :, 256], in_=u4), ["u4"], [uVk])

                def s2(t):
                    i2 = t % 2
                    eBo = eBc[i2]
                    eB = sm[:, eBo:eBo + 4]
                    Eo = E3c[t % 3]
                    Ecur = sm[:, Eo:Eo + 4]
                    Epo = E3c[(t - 1) % 3]
                    qk = qkT[i2]; qkk = ("qkT", i2)
                    Kt = Ktok[i2]; Ktk = ("Ktok", i2)
                    uVc = uV[i2]; uVk = ("uV", i2)
                    T2c = T2[i2]; T3c = T3[i2]
                    pS, pSk = nextf()
                    for h in range(4):
                        mm(pS[:, h * 128:(h + 1) * 128], qk[:, 4 + h, :], qk[:, h, :], True, True, [qkk], [pSk])
                    OP("dve", lambda e: e.tensor_tensor(
                        out=PT[:], in0=pS[:].rearrange("p (h t) -> p h t", h=4),
                        in1=triub[:].unsqueeze(1).to_broadcast([128, 4, 128]), op=ALU.mult), [pSk, "triub"], ["PT"])
                    pas = []
                    for h in range(4):
                        pa, pak = nextf()
                        pas.append((pa, pak))
                        mm(pa[:, 0:257], PT[:, h, :], uVc[:, h, 0:257], True, t == 0, ["PT", uVk], [pak])
                        if t > 0:
                            mm(pa[:, 0:257], qk[:, h, :], Cb[:, h, 0:257], False, True, [qkk, "Cb"], [pak])
                        OP("dve", lambda e, h=h, pa=pa: e.tensor_tensor(out=sm[:, 24 + h:25 + h],
                                                                       in0=sm[:, eBo + h:eBo + h + 1],
                                                                       in1=pa[:, 256:257], op=ALU.mult),
                           [pak, ("eB", i2)], [("t1", h)])
                    pus = []
                    for h in range(2):
                        pu, puk = nextf()
                        pus.append((pu, puk))
                        mm(pu[:, 0:257], Kt[:, h * 128:(h + 1) * 128], uVc[:, h, 0:257], True, True, [Ktk, uVk], [puk])
                    t1k = [("t1", h) for h in range(4)]
                    OP("act", lambda e: e.activation(out=t1, in_=t1, func=AF.Abs), t1k, t1k)
                    OP("dve", lambda e: e.tensor_scalar(out=t1, in0=t1, scalar1=1.0, scalar2=None, op0=ALU.max),
                       t1k, t1k)
                    OP("dve", lambda e: e.reciprocal(out=t1, in_=t1), t1k, t1k)
                    OP("dve", lambda e: e.tensor_tensor(out=r4, in0=eB, in1=t1, op=ALU.mult),
                       t1k + [("eB", i2)], ["r4"])
                    for h in range(4):
                        pa, pak = pas[h]
                        hsl = slice(h * 256, (h + 1) * 256)
                        OP("dve", lambda e, h=h, pa=pa, hsl=hsl: e.scalar_tensor_tensor(
                            out=ho[:, hsl], in0=pa[:, 0:256], scalar=sm[:, 28 + h:29 + h], in1=T2c[:, hsl],
                            op0=ALU.mult, op1=ALU.mult), [pak, "r4", ("T2", i2, h // 2)], [("ho", h)])
                        OP("act", lambda e, h=h, hsl=hsl: e.activation(out=bufs["junk"][:, 0:256], in_=ho[:, hsl],
                                                                      func=AF.Square, accum_out=sm[:, 32 + h:33 + h]),
                           [("ho", h)], ["junk", ("ssh", h)])
                    for h in range(2, 4):
                        pu, puk = nextf()
                        pus.append((pu, puk))
                        mm(pu[:, 0:257], Kt[:, h * 128:(h + 1) * 128], uVc[:, h, 0:257], True, True, [Ktk, uVk], [puk])
                    for h in range(4):
                        pu, puk = pus[h]
                        if t == 0:
                            OP("dve", lambda e, h=h, pu=pu: e.tensor_copy(out=Dst[:, h, :], in_=pu[:, 0:257]),
                               [puk], [("D", h)])
                        else:
                            OP("dve", lambda e, h=h, pu=pu: e.scalar_tensor_tensor(
                                out=Dst[:, h, :], in0=Dst[:, h, :], scalar=sm[:, Epo + h:Epo + h + 1], in1=pu[:, 0:257],
                                op0=ALU.mult, op1=ALU.add), [puk, ("D", h), ("E", (t - 1) % 3)], [("D", h)])
                        OP("act", lambda e, h=h: e.activation(out=Cb[:, h, 0:257], in_=Dst[:, h, :],
                                                              func=AF.Copy, scale=sm[:, Eo + h:Eo + h + 1]),
                           [("D", h), ("E", t % 3)], ["Cb"])
                    sshk = [("ssh", h) for h in range(4)]
                    OP("act", lambda e: e.activation(out=rsh, in_=ssh, func=AF.Ln, scale=1.0 / 256, bias=EPS),
                       sshk, ["rsh"])
                    OP("act", lambda e: e.activation(out=rsh, in_=rsh, func=AF.Exp, scale=-0.5), ["rsh"], ["rsh"])
                    for h in range(4):
                        hsl = slice(h * 256, (h + 1) * 256)
                        OP("dve", lambda e, h=h, hsl=hsl: e.scalar_tensor_tensor(
                            out=yb[:, hsl], in0=ho[:, hsl], scalar=sm[:, 36 + h:37 + h], in1=T3c[:, hsl],
                            op0=ALU.mult, op1=ALU.mult), [("ho", h), "rsh", ("T3", i2, h // 2)], [("yb", h)])

                def s3(t):
                    hs = t % 3
                    X = bufs["ht"][hs]
                    hk = ("ht", hs)
                    pb, pbk = nextb()
                    for k in range(8):
                        OP("pe", lambda e, k=k: e.transpose(out=pb[:, k * 128:(k + 1) * 128],
                                                            in_=yb[:, k * 128:(k + 1) * 128], identity=identb[:]),
                           [("yb", k // 2), "identb"], [pbk])
                    OP("act", lambda e: e.activation(out=yT[:].rearrange("p k t -> p (k t)"), in_=pb[:],
                                                     func=AF.Copy), [pbk], ["yT"])
                    for nb in range(2):
                        sl = slice(nb * 512, (nb + 1) * 512)
                        pf, pfk = nextf()
                        for k in range(8):
                            mm(pf[:], yT[:, k, :], Wout[:, k, sl], k == 0, k == 7, ["yT"] + wk("Wout"), [pfk])
                        OP("dve", lambda e, sl=sl, pf=pf: e.tensor_tensor(out=X[:, sl], in0=X[:, sl], in1=pf[:],
                                                                         op=ALU.add), [pfk, hk], [hk])
                    ple_and_store(bufs, l, t, hdst)

                load_h(bufs, hsrc, 0)
                if ntiles > 1:
                    load_h(bufs, hsrc, 1)
                load_p(bufs, l, 0)
                s0(0)
                s1(0)
                if ntiles > 1:
                    s0(1)
                for t in range(ntiles):
                    if t + 2 < ntiles:
                        load_h(bufs, hsrc, t + 2)
                    if t + 1 < ntiles:
                        load_p(bufs, l, t + 1)
                        s1(t + 1)
                    s2(t)
                    if t + 2 < ntiles:
                        s0(t + 2)
                    s3(t)
                S.barrier()

        SCALE = 128.0 ** -0.5
        AX = mybir.AxisListType.X

        def headnorm_rope(tb, pf, pfk, nrm_col, t, outv, outk):
            tid = tb.get("id", 0)
            sq, qf, ssq, rsq, ta, tbb, cosT, sinT = (tb["sq"], tb["qf"], tb["ssq"], tb["rsq"], tb["ta"], tb["tb"],
                                                     tb["cos"], tb["sin"])
            ksq, kqf, kss, krs, kta, ktb = [(n, tid) for n in ("sq", "qf", "ssq", "rsq", "ta", "tb")]
            pv = pf[:].rearrange("p (h d) -> p h d", h=4)
            qv = qf[:].rearrange("p (h d) -> p h d", h=4)
            OP("act", lambda e: e.activation(out=sq[:], in_=pf[:], func=AF.Square), [pfk], [ksq])
            OP("dve", lambda e: e.tensor_reduce(out=ssq[:], in_=sq[:].rearrange("p (h d) -> p h d", h=4), axis=AX,
                                                op=ALU.add), [ksq], [kss])
            OP("act", lambda e: e.activation(out=rsq[:], in_=ssq[:], func=AF.Ln, scale=1.0 / 128, bias=EPS),
               [kss], [krs])
            OP("act", lambda e: e.activation(out=rsq[:], in_=rsq[:], func=AF.Exp, scale=-0.5), [krs], [krs])
            OP("dve", lambda e: e.tensor_tensor(out=qv, in0=pv, in1=rsq[:].unsqueeze(2).to_broadcast([128, 4, 128]),
                                                op=ALU.mult), [pfk, krs], [kqf])
            OP("dve", lambda e: e.tensor_tensor(
                out=qv, in0=qv, in1=rows[:, nrm_col:nrm_col + 128].unsqueeze(1).to_broadcast([128, 4, 128]),
                op=ALU.mult), [kqf, "rows"], [kqf])
            cb_ = cosT[:, t, :].unsqueeze(1).to_broadcast([128, 4, 64])
            sb_ = sinT[:, t, :].unsqueeze(1).to_broadcast([128, 4, 64])
            x1 = qv[:, :, 0:64]
            x2 = qv[:, :, 64:128]
            OP("dve", lambda e: e.tensor_tensor(out=ta[:], in0=x1, in1=cb_, op=ALU.mult), [kqf, "cs"], [kta])
            OP("dve", lambda e: e.tensor_tensor(out=tbb[:], in0=x2, in1=sb_, op=ALU.mult), [kqf, "cs"], [ktb])
            OP("dve", lambda e: e.tensor_tensor(out=outv[:, :, 0:64], in0=ta[:], in1=tbb[:], op=ALU.subtract),
               [kta, ktb], [outk])
            OP("dve", lambda e: e.tensor_tensor(out=ta[:], in0=x2, in1=cb_, op=ALU.mult), [kqf, "cs"], [kta])
            OP("dve", lambda e: e.tensor_tensor(out=tbb[:], in0=x1, in1=sb_, op=ALU.mult), [kqf, "cs"], [ktb])
            OP("dve", lambda e: e.tensor_tensor(out=outv[:, :, 64:128], in0=ta[:], in1=tbb[:], op=ALU.add),
               [kta, ktb], [outk])

        def phase_B1(l, j, hsrc, KT, VT):
            S.new_epoch()
            S.barrier()
            es = contextlib.ExitStack()
            with es:
                bufs = {}
                Wq = sb(es, "Wq", [128, 8, 1536], BF16)
                specs = [(Wq, w_in_b_d[j][:, :, 0:1536], 8, 1536, CV[("norm_b", j)], "Wq")]
                if j == 0:
                    Wkv = sb(es, "Wkv", [128, 8, 1024], BF16)
                    specs.append((Wkv, w_kv_d, 8, 1024, CV["norm_kv"], "Wkv"))
                load_weights(specs)
                bufs["ht"] = [sb(es, "ht%d" % i, [128, D], F32) for i in range(3)]
                bufs["junk"] = sb(es, "junk", [128, D], BF16)
                bufs["ss"] = sb(es, "ss", [128, 2], F32)
                bufs["rstd"] = sb(es, "rstd", [128, 2], F32)
                bufs["xs"] = [sb(es, "xs%d" % i, [128, D], BF16) for i in range(2)]
                bufs["xsT"] = [sb(es, "xsT%d" % i, [128, 8, 128], BF16) for i in range(2)]
                cosT = sb(es, "cosT", [128, NT, 64], F32)
                sinT = sb(es, "sinT", [128, NT, 64], F32)
                tbs = []
                for i in range(2):
                    tb = {"id": i, "cos": cosT, "sin": sinT}
                    tb["sq"] = sb(es, "sq", [128, 512], F32)
                    tb["qf"] = sb(es, "qf", [128, 512], F32)
                    tb["ssq"] = sb(es, "ssq", [128, 4], F32)
                    tb["rsq"] = sb(es, "rsq", [128, 4], F32)
                    tb["ta"] = sb(es, "ta", [128, 4, 64], F32)
                    tb["tb"] = sb(es, "tb", [128, 4, 64], F32)
                    tbs.append(tb)
                qr = [sb(es, "qr%d" % i, [128, 12, 128], BF16) for i in range(2)]
                kr = [sb(es, "kr%d" % i, [128, 4, 128], BF16) for i in range(2)]
                qTs = [sb(es, "qTs%d" % i, [128, 12, 128], BF16) for i in range(2)]
                S.dma("sp", "cs", [lambda e: e.dma_start(out=cosT[:], in_=cos_d),
                                   lambda e: e.dma_start(out=sinT[:], in_=sin_d)], writes=["cs"])

                def ld(t):
                    hs = t % 3
                    S.dma("sp", ("ht", hs),
                          [lambda e: e.dma_start(out=bufs["ht"][hs][:], in_=hsrc[t * 128:(t + 1) * 128, :])],
                          writes=[("ht", hs)])
                qn = RW[("qnorm", j)]
                ctx = {}
                chain = [0]

                def Sa(t):
                    hs = t % 3
                    ctx[("x", t)] = norm_T(bufs, bufs["ht"][hs][:], ("ht", hs), slot=t % 2)

                def Sb(t):
                    xT, xTk = ctx.pop(("x", t))
                    banks = {}
                    if j == 0:
                        pf, pfk = nextf()
                        for c in range(4):
                            for k in range(8):
                                mm(pf[:, c * 128:(c + 1) * 128], Wkv[:, k, 512 + c * 128:512 + (c + 1) * 128],
                                   xT[:, k, :], k == 0, k == 7, [xTk] + wk("Wkv"), [pfk])
                        banks["v"] = (pf, pfk)
                        pf, pfk = nextf()
                        for k in range(8):
                            mm(pf[:], xT[:, k, :], Wkv[:, k, 0:512], k == 0, k == 7, [xTk] + wk("Wkv"), [pfk])
                        banks["k"] = (pf, pfk)
                    for g in range(3):
                        pf, pfk = nextf()
                        for k in range(8):
                            mm(pf[:], xT[:, k, :], Wq[:, k, g * 512:(g + 1) * 512], k == 0, k == 7,
                               [xTk] + wk("Wq"), [pfk])
                        banks[g] = (pf, pfk)
                    ctx[("b", t)] = banks

                def hr(pf, pfk, col, t, outv, outk):
                    tb = tbs[chain[0] % 2]; chain[0] += 1
                    headnorm_rope(tb, pf, pfk, col, t, outv, outk)

                def Sc(t):
                    banks = ctx.pop(("b", t))
                    tsl = slice(t * 128, (t + 1) * 128)
                    i2 = t % 2
                    if j == 0:
                        pfv, pfvk = banks["v"]
                        OP("act", lambda e: e.activation(
                            out=VT[:, :, tsl], in_=pfv[:].rearrange("p (c t) -> p c t", c=4), func=AF.Copy),
                           [pfvk], ["VT"])
                        pf, pfk = banks["k"]
                        hr(pf, pfk, RW["knorm"], t, kr[i2][:], ("kr", i2))
                    for g in range(3):
                        pf, pfk = banks[g]
                        hr(pf, pfk, qn, t, qr[i2][:, g * 4:(g + 1) * 4, :], ("qr", i2, g))

                def Sd(t):
                    tsl = slice(t * 128, (t + 1) * 128)
                    i2 = t % 2
                    if j == 0:
                        pb, pbk = nextb()
                        for h in range(4):
                            OP("pe", lambda e, h=h: e.transpose(out=pb[:, h * 128:(h + 1) * 128], in_=kr[i2][:, h, :],
                                                                identity=identb[:]), [("kr", i2), "identb"], [pbk])
                        OP("act", lambda e: e.activation(
                            out=KT[:, :, tsl], in_=pb[:, 0:512].rearrange("p (c t) -> p c t", c=4), func=AF.Copy),
                           [pbk], ["KT"])
                    qs = qTs[i2]
                    qsk = ("qTs", i2)
                    for half in range(2):
                        nh = 8 if half == 0 else 4
                        pb2, pb2k = nextb()
                        for hh in range(nh):
                            gh = half * 8 + hh
                            OP("pe", lambda e, hh=hh, gh=gh, pb2=pb2: e.transpose(
                                out=pb2[:, hh * 128:(hh + 1) * 128], in_=qr[i2][:, gh, :], identity=identb[:]),
                               [("qr", i2, gh // 4), "identb"], [pb2k])
                        OP("act", lambda e, pb2=pb2, half=half, nh=nh: e.activation(
                            out=qs[:, half * 8:half * 8 + nh, :],
                            in_=pb2[:, 0:nh * 128].rearrange("p (c t) -> p c t", c=nh), func=AF.Copy),
                           [pb2k], [(qsk, half)])
                    S.dma("pool", ("qst", i2),
                          [lambda e: e.dma_start(
                              out=qT_d[:, :, :, tsl].rearrange("h d g t -> d g h t"),
                              in_=qs[:].rearrange("p (g h) t -> p g h t", g=3))],
                          reads=[(qsk, 0), (qsk, 1)], writes=["qT_d"])

                for t in range(min(3, ntiles)):
                    ld(t)
                Sa(0)
                if ntiles > 1:
                    Sa(1)
                Sb(0)
                for t in range(ntiles):
                    Sc(t)
                    if t + 3 < ntiles:
                        ld(t + 3)
                    if t + 2 < ntiles:
                        Sa(t + 2)
                    if t + 1 < ntiles:
                        Sb(t + 1)
                    Sd(t)
                S.barrier()

        def phase_B2(KT, VT, oT):
            S.new_epoch()
            S.barrier()
            es = contextlib.ExitStack()
            with es:
                QTh = sb(es, "QTh", [128, 3, S_LEN], BF16)
                Vg = [sb(es, "Vg%d" % g, [128, 32, 128], BF16) for g in range(3)]
                num = sb(es, "num", [128, S_LEN], F32)
                den = sb(es, "den", [128, S_LEN], F32)
                PTa = [sb(es, "PTa%d" % i, [128, 4, 128], BF16) for i in range(4)]
                pc = [0]
                for h in range(4):
                    S.dma("sp", "qld", [lambda e, h=h: e.dma_start(out=QTh[:], in_=qT_d[h])],
                          reads=["qT_d"], writes=["QTh"])
                    for g, r in enumerate((1, 4, 16)):
                        nbk = 32 // r
                        for b0 in range(0, 32, 8):
                            pb, pbk = nextb()
                            for bb in range(8):
                                blk = b0 + bb
                                c, n = blk // nbk, blk % nbk
                                base = c + r * 128 * n
                                OP("pe", lambda e, pb=pb, bb=bb, h=h, base=base, r=r: e.transpose(
                                    out=pb[:, bb * 128:(bb + 1) * 128],
                                    in_=VT[:, h, base:base + r * 127 + 1:r], identity=identb[:]),
                                   ["VT", "identb"], [pbk])
                            OP("act", lambda e, pb=pb, g=g, b0=b0: e.activation(
                                out=Vg[g][:, b0:b0 + 8, :], in_=pb[:].rearrange("p (c t) -> p c t", c=8),
                                func=AF.Copy), [pbk], [("Vg", g)])
                    items = []
                    for g, r in enumerate((1, 4, 16)):
                        if g == 0:
                            for m in range(8):
                                items.append((g, r, [(0, 4 * m + jb) for jb in range(4)],
                                              lambda a, m=m: a[:, 512 * m:512 * (m + 1)].rearrange(
                                                  "p (j i) -> p j i", j=4)))
                        elif g == 1:
                            for n in range(8):
                                items.append((g, r, [(c, n) for c in range(4)],
                                              lambda a, n=n: a[:, 512 * n:512 * (n + 1)].rearrange(
                                                  "p (i c) -> p c i", c=4)))
                        else:
                            for n in range(2):
                                for m in range(4):
                                    items.append((g, r, [(4 * m + jb, n) for jb in range(4)],
                                                  lambda a, n=n, m=m: a[:, 2048 * n:2048 * (n + 1)].rearrange(
                                                      "p (i c) -> p c i", c=16)[:, 4 * m:4 * m + 4, :]))

                    def P(item):
                        g, r, blocks, view = item
                        pts = []
                        for half in range(2):
                            ps_, psk = nextf()
                            pt_ = PTa[pc[0] % 4]
                            ptk = ("PTa", pc[0] % 4)
                            pc[0] += 1
                            for q2 in range(2):
                                c, n = blocks[half * 2 + q2]
                                b_ = c + r * 128 * n
                                qsl = slice(b_, b_ + r * 127 + 1, r)
                                mm(ps_[:, q2 * 256:q2 * 256 + 128], KT[:, h, qsl], QTh[:, g, qsl], True, True,
                                   ["KT", "QTh"], [psk])
                                if n >= 1:
                                    bp = c + r * 128 * (n - 1)
                                    psl = slice(bp, bp + r * 127 + 1, r)
                                    mm(ps_[:, q2 * 256 + 128:q2 * 256 + 256], KT[:, h, psl], QTh[:, g, qsl],
                                       True, True, ["KT", "QTh"], [psk])
                            OP("act", lambda e, ps_=ps_, pt_=pt_: e.activation(
                                out=pt_[:].rearrange("p a b -> p (a b)"), in_=ps_[:], func=AF.Exp, scale=SCALE),
                               [psk], [ptk])
                            OP("dve", lambda e, pt_=pt_: e.tensor_tensor(out=pt_[:], in0=pt_[:], in1=mask2[:],
                                                                        op=ALU.mult), [ptk, "mask2"], [ptk])
                            pts.append((pt_, ptk))
                        return pts

                    def Q(item, pts):
                        g, r, blocks, view = item
                        nbk = 32 // r
                        pO, pOk = nextf()
                        pD, pDk = nextf()
                        for jb in range(4):
                            c, n = blocks[jb]
                            pt_, ptk = pts[jb // 2]
                            q2 = jb % 2
                            bi = c * nbk + n
                            osl = slice(jb * 128, (jb + 1) * 128)
                            mm(pO[:, osl], Vg[g][:, bi, :], pt_[:, q2 * 2, :], True, n == 0,
                               [("Vg", g), ptk], [pOk])
                            if n >= 1:
                                mm(pO[:, osl], Vg[g][:, bi - 1, :], pt_[:, q2 * 2 + 1, :], False, True,
                                   [("Vg", g), ptk], [pOk])
                            mm(pD[:, osl], onesb[:], pt_[:, q2 * 2, :], True, n == 0, ["onesb", ptk], [pDk])
                            if n >= 1:
                                mm(pD[:, osl], onesb[:], pt_[:, q2 * 2 + 1, :], False, True, ["onesb", ptk], [pDk])
                        nv = view(num)
                        dv = view(den)
                        pOv = pO[:].rearrange("p (j i) -> p j i", j=4)
                        pDv = pD[:].rearrange("p (j i) -> p j i", j=4)
                        if g == 0:
                            OP("act", lambda e: e.activation(out=nv, in_=pOv, func=AF.Copy), [pOk], ["num"])
                            OP("dve", lambda e: e.tensor_copy(out=dv, in_=pDv), [pDk], ["den"])
                        else:
                            OP("dve", lambda e: e.tensor_tensor(out=nv, in0=nv, in1=pOv, op=ALU.add),
                               [pOk, "num"], ["num"])
                            OP("dve", lambda e: e.tensor_tensor(out=dv, in0=dv, in1=pDv, op=ALU.add),
                               [pDk, "den"], ["den"])

                    cur = P(items[0])
                    for bi_ in range(len(items)):
                        nxt = P(items[bi_ + 1]) if bi_ + 1 < len(items) else None
                        Q(items[bi_], cur)
                        cur = nxt
                    OP("act", lambda e: e.activation(out=den[:], in_=den[:], func=AF.Ln), ["den"], ["den"])
                    OP("act", lambda e: e.activation(out=den[:], in_=den[:], func=AF.Exp, scale=-1.0), ["den"], ["den"])
                    OP("dve", lambda e, h=h: e.tensor_tensor(out=oT[:, h, :], in0=num[:], in1=den[:], op=ALU.mult),
                       ["num", "den"], ["oT"])
                S.barrier()

        def phase_B3(l, j, hsrc, hdst, oT):
            S.new_epoch()
            S.barrier()
            es = contextlib.ExitStack()
            with es:
                bufs = alloc_ple_w(es)
                Wz = sb(es, "Wz", [128, 8, 512], BF16)
                Wob = sb(es, "Wob", [128, 4, 1024], BF16)
                load_weights([(Wz, w_in_b_d[j][:, :, 1536:2048], 8, 512, CV[("norm_b", j)], "Wz"),
                              (Wob, w_out_b_d[j], 4, 1024, None, "Wob"),
                              (bufs["Wg"], w_pg_d[l], 8, 1024, CV[("ple_norm", l)], "Wg"),
                              (bufs["Wp"], w_pp_d[l], 2, 1024, None, "Wp")], nstg=3)
                alloc_common(es, bufs)
                Tz = sb(es, "Tz", [128, 512], F32)
                yTs = [sb(es, "yTb%d" % i, [128, 4, 128], BF16) for i in range(2)]
                ctx = {}

                def s0(t):
                    hs = t % 3
                    ctx[t] = norm_T(bufs, bufs["ht"][hs][:], ("ht", hs), slot=t % 2)

                def s1(t):
                    xT, xTk = ctx.pop(t)
                    tsl = slice(t * 128, (t + 1) * 128)
                    yT = yTs[t % 2]
                    pf, pfk = nextf()
                    for c in range(4):
                        for k in range(8):
                            mm(pf[:, c * 128:(c + 1) * 128], Wz[:, k, c * 128:(c + 1) * 128], xT[:, k, :],
                               k == 0, k == 7, [xTk] + wk("Wz"), [pfk])
                    sigmoid_inplace(Tz[:], pf[:], [pfk], "Tz")
                    OP("dve", lambda e: e.tensor_tensor(out=Tz[:], in0=pf[:], in1=Tz[:], op=ALU.mult),
                       [pfk, "Tz"], ["Tz"])
                    OP("dve", lambda e: e.tensor_tensor(
                        out=yT[:], in0=Tz[:].rearrange("p (c t) -> p c t", c=4), in1=oT[:, :, tsl], op=ALU.mult),
                       ["Tz", "oT"], [("yTb", t % 2)])

                def s3(t):
                    hs = t % 3
                    X = bufs["ht"][hs]
                    hk = ("ht", hs)
                    yT = yTs[t % 2]
                    for nb in range(2):
                        sl = slice(nb * 512, (nb + 1) * 512)
                        pf2, pf2k = nextf()
                        for c in range(4):
                            mm(pf2[:], yT[:, c, :], Wob[:, c, sl], c == 0, c == 3,
                               [("yTb", t % 2)] + wk("Wob"), [pf2k])
                        OP("dve", lambda e, sl=sl, pf2=pf2: e.tensor_tensor(out=X[:, sl], in0=X[:, sl], in1=pf2[:],
                                                                           op=ALU.add), [pf2k, hk], [hk])
                    ple_and_store(bufs, l, t, hdst)

                load_h(bufs, hsrc, 0)
                if ntiles > 1:
                    load_h(bufs, hsrc, 1)
                load_p(bufs, l, 0)
                s0(0)
                s1(0)
                if ntiles > 1:
                    s0(1)
                for t in range(ntiles):
                    if t + 2 < ntiles:
                        load_h(bufs, hsrc, t + 2)
                    if t + 1 < ntiles:
                        load_p(bufs, l, t + 1)
                        s1(t + 1)
                    if t + 2 < ntiles:
                        s0(t + 2)
                    s3(t)
                S.barrier()

        srcs = [x_d, hA_d, hB_d, hA_d]
        dsts = [hA_d, hB_d, hA_d, out_d]
        dsts[n_layers - 1] = out_d
        kvs = contextlib.ExitStack()
        KT = VT = None
        for l in range(n_layers):
            if l < 2:
                layer_A(l, srcs[l], dsts[l])
            else:
                j = l - 2
                if j == 0:
                    KT = sb(kvs, "KT", [128, 4, S_LEN], BF16)
                    VT = sb(kvs, "VT", [128, 4, S_LEN], BF16)
                    if ntiles < NT:
                        OP("pool", lambda e: e.memset(KT[:], 0.0), [], ["KT"])
                        OP("pool", lambda e: e.memset(VT[:], 0.0), [], ["VT"])
                phase_B1(l, j, srcs[l], KT, VT)
                with contextlib.ExitStack() as os_:
                    oT = sb(os_, "oT", [128, 4, S_LEN], BF16)
                    phase_B2(KT, VT, oT)
                    phase_B3(l, j, srcs[l], dsts[l], oT)
        kvs.close()
        finals = [o for k, o in S.last.items() if isinstance(k, tuple) and k[0] == "dma" and
                  isinstance(k[1], tuple) and k[1][0] == "st"]
        S.finalize_and_emit(final_waits=finals)
    return nc, S


def _wl(w):
    w = np.asarray(w, np.float32)
    return np.ascontiguousarray(w.reshape(-1, 128, w.shape[-1]).transpose(1, 0, 2))


def host_inputs(inp):
    g = {}
    g["w_in_a"] = np.stack([_wl(inp["w_in_a"][l]) for l in range(2)])
    g["w_out_a"] = np.stack([_wl(inp["w_out_a"][l]) for l in range(2)])
    g["w_kv"] = _wl(inp["w_kv"])
    g["w_in_b"] = np.stack([_wl(inp["w_in_b"][l]) for l in range(2)])
    g["w_out_b"] = np.stack([_wl(inp["w_out_b"][l]) for l in range(2)])
    g["w_ple_gate"] = np.stack([_wl(inp["w_ple_gate"][l]) for l in range(4)])
    g["w_ple"] = np.stack([_wl(inp["w_ple"][l]) for l in range(4)])
    cv = np.zeros((128, NCV), np.float32)
    for l in range(2):
        c = CV[("norm_a", l)]; cv[:, c:c + 8] = _pk(inp["norm_a"][l])
        c = CV[("hnorm_a", l)]; cv[:, c:c + 8] = _pk(inp["hnorm_a"][l])
        c = CV[("conv_a", l)]
        for j in range(4):
            cv[:, c + j * 8:c + j * 8 + 8] = _pk(inp["conv_a"][l][j])
        c = CV[("norm_b", l)]; cv[:, c:c + 8] = _pk(inp["norm_b"][l])
    c = CV["norm_kv"]; cv[:, c:c + 8] = _pk(inp["norm_kv"])
    for l in range(4):
        c = CV[("ple_norm", l)]; cv[:, c:c + 8] = _pk(inp["ple_norm"][l])
    g["cvec"] = cv
    rw = np.zeros((128, NRW), np.float32)
    for l in range(2):
        c = RW[("b_gate", l)]; rw[:, c:c + 8] = np.asarray(inp["b_gate_a"][l], np.float32)[None, :]
        c = RW[("qnorm", l)]; rw[:, c:c + 128] = np.asarray(inp["qnorm_b"][l], np.float32)[None, :]
    c = RW["knorm"]; rw[:, c:c + 128] = np.asarray(inp["knorm"], np.float32)[None, :]
    g["rows"] = rw
    inv = (np.float32(10000.0) ** (-np.arange(0, 128, 2, dtype=np.float32) / np.float32(128))).astype(np.float32)
    ang = (np.arange(S_LEN, dtype=np.float32)[:, None] * inv[None, :]).astype(np.float32)
    cos = np.cos(ang).astype(np.float32).reshape(NT, 128, 64).transpose(1, 0, 2)
    sin = np.sin(ang).astype(np.float32).reshape(NT, 128, 64).transpose(1, 0, 2)
    g["cos"] = np.ascontiguousarray(cos)
    g["sin"] = np.ascontiguousarray(sin)
    cst = np.zeros((128, 4, 128), np.float32)
    cst[:, 0, :] = np.eye(128, dtype=np.float32)
    cst[:, 1, :] = np.triu(np.ones((128, 128), np.float32))
    cst[:, 2, :] = np.tril(np.ones((128, 128), np.float32))
    cst[:, 3, :] = 1.0
    g["cst"] = cst
    return g


_CACHE = {}


def kernel(**inputs):
    inp = {k: np.asarray(v) for k, v in inputs.items()}
    B = inp["x"].shape[0]
    if "prog" not in _CACHE:
        _CACHE["prog"] = build_program(4, NT)[0]
    nc = _CACHE["prog"]
    g = host_inputs(inp)
    in_maps = []
    for b in range(B):
        m = dict(g)
        m["x"] = np.ascontiguousarray(inp["x"][b], dtype=np.float32)
        m["p"] = np.ascontiguousarray(inp["p"][:, b], dtype=np.float32)
        in_maps.append(m)
    res = run_bass_kernel_spmd(nc, in_maps, core_ids=list(range(B)))
    out = np.stack([np.asarray(r["out"], np.float32) for r in res.results], axis=0)
    return out
```

```python
import contextlib
import math
import numpy as np
import concourse.bass as bass
import concourse.mybir as mybir
from concourse.bass_utils import run_bass_kernel_spmd

F32 = mybir.dt.float32
BF16 = mybir.dt.bfloat16
AF = mybir.ActivationFunctionType
ALU = mybir.AluOpType

S_LEN = 4096
NT = S_LEN // 128
D = 1024
EPS = 1e-6
ENGS = ("pe", "act", "dve", "pool", "sp")


class Op:
    __slots__ = ("eng", "fn", "deps", "need_ms", "ms", "dom", "is_dma", "chan",
                 "ndma", "dval", "clock", "waits", "epoch")


class Sched:
    def __init__(self, nc, same_engine_sync=True):
        self.nc = nc
        self.ops = {e: [] for e in ENGS}
        self.order = []
        self.state = {}
        self.epoch = 0
        self.same_engine_sync = same_engine_sync
        self.chan_count = {}
        self.chans = []
        self.last = {}
        self.pending = {e: [] for e in ENGS}

    def new_epoch(self):
        self.epoch += 1

    def barrier(self):
        deps = list(self.last.values())
        for e in ENGS:
            self.pending[e] = list(deps)

    def _deps_for(self, eng, reads, writes):
        deps = []
        for k in reads:
            st = self.state.get(k)
            if st is not None and st[0] is not None:
                deps.append(st[0])
        for k in writes:
            st = self.state.get(k)
            if st is not None:
                if st[0] is not None:
                    deps.append(st[0])
                lastr = {}
                for r_ in st[1]:
                    if r_.is_dma:
                        deps.append(r_)
                    else:
                        lastr[(r_.eng, r_.epoch)] = r_
                deps.extend(lastr.values())
        if self.pending[eng]:
            deps.extend(self.pending[eng])
            self.pending[eng] = []
        return deps

    def _commit(self, op, reads, writes):
        for k in reads:
            st = self.state.setdefault(k, [None, []])
            st[1].append(op)
        for k in writes:
            self.state[k] = [op, []]

    def op(self, eng, fn, reads=(), writes=()):
        o = Op()
        o.eng = eng; o.fn = fn; o.need_ms = False; o.ms = None
        o.is_dma = False; o.epoch = self.epoch
        o.deps = self._deps_for(eng, reads, writes)
        self._commit(o, reads, writes)
        self.ops[eng].append(o)
        self.order.append(o)
        self.last[eng] = o
        return o

    def dma(self, eng, chan, fns, reads=(), writes=()):
        o = Op()
        o.eng = eng; o.fn = fns; o.need_ms = False; o.ms = None
        o.is_dma = True; o.chan = chan; o.ndma = len(fns); o.epoch = self.epoch
        if chan not in self.chan_count:
            self.chan_count[chan] = 0
            self.chans.append(chan)
        self.chan_count[chan] += len(fns)
        o.dval = 16 * self.chan_count[chan]
        o.deps = self._deps_for(eng, reads, writes)
        self._commit(o, reads, writes)
        self.ops[eng].append(o)
        self.order.append(o)
        self.last[("dma", chan)] = o
        return o

    def _skip(self, d, o):
        return (not d.is_dma) and d.eng == o.eng and (not o.is_dma) and \
            (o.eng == "pe" or not self.same_engine_sync)

    def finalize_and_emit(self, final_waits=()):
        nc = self.nc
        for o in self.order:
            for d in o.deps:
                if d.is_dma or self._skip(d, o):
                    continue
                d.need_ms = True
        cnt = {}
        for o in self.order:
            if o.is_dma:
                o.dom = ("dma", o.chan)
                continue
            o.dom = (o.eng, o.epoch)
            if o.need_ms:
                cnt[o.dom] = cnt.get(o.dom, 0) + 1
                o.ms = cnt[o.dom]
        eclock = {e: {} for e in ENGS}
        nwaits = 0
        for o in self.order:
            ck = eclock[o.eng]
            waits = {}
            for d in o.deps:
                if d.is_dma:
                    dom, val = d.dom, d.dval
                else:
                    if self._skip(d, o):
                        continue
                    dom, val = d.dom, d.ms
                if ck.get(dom, 0) >= val:
                    continue
                if waits.get(dom, 0) < val:
                    waits[dom] = val
            for d in o.deps:
                if d.is_dma:
                    dom, val = d.dom, d.dval
                else:
                    dom, val = d.dom, d.ms
                if dom in waits and waits[dom] == val and d.clock is not None:
                    for k, v in d.clock.items():
                        if ck.get(k, 0) < v:
                            ck[k] = v
            for dom, val in waits.items():
                if ck.get(dom, 0) < val:
                    ck[dom] = val
            o.waits = list(waits.items())
            nwaits += len(o.waits)
            if o.is_dma:
                c = dict(ck); c[o.dom] = o.dval
                o.clock = c
            elif o.ms is not None:
                c = dict(ck); c[o.dom] = o.ms
                if o.eng == "pe" or not self.same_engine_sync:
                    ck[o.dom] = o.ms
                o.clock = c
            else:
                o.clock = None
        self.nwaits = nwaits
        stack = contextlib.ExitStack()
        sems = {}
        doms = list(cnt.keys()) + [("dma", c) for c in self.chans]
        for i, dom in enumerate(doms):
            sems[dom] = stack.enter_context(nc.semaphore("s%d" % i))
        fw = [(sems[d.dom], d.dval) for d in final_waits]
        ops = self.ops
        with stack:
            with nc.Block() as block:
                def emit(engname, engobj, last=False):
                    for o in ops[engname]:
                        for dom, val in o.waits:
                            engobj.wait_ge(sems[dom], val)
                        if o.is_dma:
                            for f in o.fn:
                                f(engobj).then_inc(sems[o.dom], 16)
                        else:
                            ins = o.fn(engobj)
                            if o.ms is not None:
                                ins.then_inc(sems[o.dom], 1)
                    if last:
                        for s, v in fw:
                            engobj.wait_ge(s, v)

                @block.tensor
                def _(e):
                    emit("pe", e)

                @block.scalar
                def _(e):
                    emit("act", e)

                @block.vector
                def _(e):
                    emit("dve", e)

                @block.gpsimd
                def _(e):
                    emit("pool", e)

                @block.sync
                def _(e):
                    emit("sp", e, last=True)


def _pk(v):
    return np.ascontiguousarray(np.asarray(v, np.float32).reshape(-1, 128).T)


CV = {}
_c = 0
for _l in range(2):
    CV[("norm_a", _l)] = _c; _c += 8
    CV[("hnorm_a", _l)] = _c; _c += 8
    CV[("conv_a", _l)] = _c; _c += 32
CV["norm_kv"] = _c; _c += 8
for _l in range(2):
    CV[("norm_b", _l)] = _c; _c += 8
for _l in range(4):
    CV[("ple_norm", _l)] = _c; _c += 8
NCV = _c
RW = {}
_c = 0
for _l in range(2):
    RW[("b_gate", _l)] = _c; _c += 8
RW["knorm"] = _c; _c += 128
for _l in range(2):
    RW[("qnorm", _l)] = _c; _c += 128
NRW = _c
LNSK = math.log(128.0 ** -0.5)


def build_program(n_layers=4, ntiles=NT):
    nc = bass.Bass("TRN2", target_bir_lowering=False)
    S_ = ntiles * 128

    def din(name, shape):
        return nc.dram_tensor(name, shape, F32, kind="ExternalInput").ap()

    x_d = din("x", [S_LEN, D])
    p_d = din("p", [4, S_LEN, 256])
    w_in_a_d = din("w_in_a", [2, 128, 8, 4104])
    w_out_a_d = din("w_out_a", [2, 128, 8, 1024])
    w_kv_d = din("w_kv", [128, 8, 1024])
    w_in_b_d = din("w_in_b", [2, 128, 8, 2048])
    w_out_b_d = din("w_out_b", [2, 128, 4, 1024])
    w_pg_d = din("w_ple_gate", [4, 128, 8, 1024])
    w_pp_d = din("w_ple", [4, 128, 2, 1024])
    cvec_d = din("cvec", [128, NCV])
    rows_d = din("rows", [128, NRW])
    cos_d = din("cos", [128, NT, 64])
    sin_d = din("sin", [128, NT, 64])
    cst_d = din("cst", [128, 4, 128])
    out_d = nc.dram_tensor("out", [S_LEN, D], F32, kind="ExternalOutput").ap()
    hA_d = nc.dram_tensor("hA", [S_LEN, D], F32).ap()
    hB_d = nc.dram_tensor("hB", [S_LEN, D], F32).ap()
    qT_d = nc.dram_tensor("qT", [4, 128, 3, S_LEN], BF16).ap()

    S = Sched(nc)
    OP = S.op
    gs = contextlib.ExitStack()

    uid = [0]

    def sb(es, name, shape, dt):
        uid[0] += 1
        return es.enter_context(nc.sbuf_tensor("%s_u%d" % (name, uid[0]), shape, dt))

    cnt = {"b": 0, "f": 0, "n": 0, "stg": 0, "cv": 0}

    with gs:
        cstf = sb(gs, "cstf", [128, 4, 128], F32)
        identb = sb(gs, "identb", [128, 128], BF16)
        triub = sb(gs, "triub", [128, 128], BF16)
        mask2 = sb(gs, "mask2", [128, 4, 128], BF16)
        onesb = sb(gs, "onesb", [128, 128], BF16)
        cvec = sb(gs, "cvec", [128, NCV], F32)
        rows = sb(gs, "rows", [128, NRW], F32)
        PSB = [gs.enter_context(nc.psum_tensor("psb%d" % i, [128, 1024], BF16)) for i in range(2)]
        PSF = [gs.enter_context(nc.psum_tensor("psf%d" % i, [128, 512], F32)) for i in range(6)]
        triuf = cstf[:, 1, :]
        onesf = cstf[:, 3, :]

        def nextb():
            i = cnt["b"] % 2; cnt["b"] += 1
            return PSB[i], ("pb", i)

        def nextf():
            i = cnt["f"] % 6; cnt["f"] += 1
            return PSF[i], ("pf", i)

        S.dma("sp", "cst", [lambda e: e.dma_start(out=cstf[:], in_=cst_d),
                            lambda e: e.dma_start(out=cvec[:], in_=cvec_d),
                            lambda e: e.dma_start(out=rows[:], in_=rows_d)],
              writes=["cstf", "cvec", "rows"])
        OP("dve", lambda e: e.tensor_copy(out=identb[:], in_=cstf[:, 0, :]), ["cstf"], ["identb"])
        OP("dve", lambda e: e.tensor_copy(out=triub[:], in_=cstf[:, 1, :]), ["cstf"], ["triub"])
        OP("dve", lambda e: e.tensor_copy(out=onesb[:], in_=cstf[:, 3, :]), ["cstf"], ["onesb"])
        for i in range(4):
            OP("dve", lambda e, i=i: e.tensor_copy(out=mask2[:, i, :], in_=cstf[:, 1 + (i % 2), :]),
               ["cstf"], ["mask2"])

        def mm(out, lhsT, rhs, start, stop, r, w):
            OP("pe", lambda e: e.matmul(out=out, lhsT=lhsT, rhs=rhs, start=start, stop=stop), r, w)

        def sigmoid_inplace(dst, src, rkeys, wkey):
            OP("act", lambda e: e.activation(out=dst, in_=src, func=AF.Exp, scale=-1.0), rkeys, [wkey])
            OP("act", lambda e: e.activation(out=dst, in_=dst, func=AF.Ln, bias=1.0), [wkey], [wkey])
            OP("act", lambda e: e.activation(out=dst, in_=dst, func=AF.Exp, scale=-1.0), [wkey], [wkey])

        def load_weights(specs, nstg=4):
            with contextlib.ExitStack() as ws:
                stg = [sb(ws, "stgL%d" % i, [128, 4104], F32) for i in range(nstg)]
                for dst, src, K, N, gcol, wkey in specs:
                    kk = max(1, 4104 // N)
                    for k0 in range(0, K, kk):
                        k1 = min(K, k0 + kk)
                        si = cnt["stg"] % nstg; cnt["stg"] += 1
                        sv = stg[si][:, 0:(k1 - k0) * N].rearrange("p (k n) -> p k n", k=k1 - k0)
                        S.dma("sp", ("stg", si),
                              [lambda e, sv=sv, src=src, k0=k0, k1=k1: e.dma_start(out=sv, in_=src[:, k0:k1, :])],
                              writes=[("stg", si)])
                        for k in range(k0, k1):
                            eng = ("act", "dve")[cnt["cv"] % 2]; cnt["cv"] += 1
                            o_ap = dst[:, k, :]
                            i_ap = sv[:, k - k0, :]
                            if gcol is None:
                                if eng == "act":
                                    fn = lambda e, o_ap=o_ap, i_ap=i_ap: e.activation(out=o_ap, in_=i_ap, func=AF.Copy)
                                else:
                                    fn = lambda e, o_ap=o_ap, i_ap=i_ap: e.tensor_copy(out=o_ap, in_=i_ap)
                            else:
                                g_ap = cvec[:, gcol + k:gcol + k + 1]
                                if eng == "act":
                                    fn = lambda e, o_ap=o_ap, i_ap=i_ap, g_ap=g_ap: e.activation(
                                        out=o_ap, in_=i_ap, func=AF.Copy, scale=g_ap)
                                else:
                                    fn = lambda e, o_ap=o_ap, i_ap=i_ap, g_ap=g_ap: e.tensor_scalar(
                                        out=o_ap, in0=i_ap, scalar1=g_ap, scalar2=None, op0=ALU.mult)
                            OP(eng, fn, [("stg", si), "cvec"], [(wkey, eng)])
                S.barrier()

        def wk(name):
            return [(name, "act"), (name, "dve")]

        def norm_T(bufs, src, skey, slot=None, dim=D):
            if slot is None:
                i = cnt["n"] % 2; cnt["n"] += 1
            else:
                i = slot
            junk, ss, rstd, xs, xsT = bufs["junk"], bufs["ss"], bufs["rstd"], bufs["xs"], bufs["xsT"]
            OP("act", lambda e: e.activation(out=junk[:], in_=src, func=AF.Square, accum_out=ss[:, i:i + 1]),
               [skey], ["junk", ("ss", i)])
            OP("act", lambda e: e.activation(out=rstd[:, i:i + 1], in_=ss[:, i:i + 1], func=AF.Ln,
                                             scale=1.0 / dim, bias=EPS), [("ss", i)], [("rstd", i)])
            OP("act", lambda e: e.activation(out=rstd[:, i:i + 1], in_=rstd[:, i:i + 1], func=AF.Exp, scale=-0.5),
               [("rstd", i)], [("rstd", i)])
            OP("dve", lambda e: e.tensor_scalar(out=xs[i][:], in0=src, scalar1=rstd[:, i:i + 1], scalar2=None,
                                                op0=ALU.mult), [skey, ("rstd", i)], [("xs", i)])
            pb, pbk = nextb()
            for k in range(8):
                OP("pe", lambda e, k=k: e.transpose(out=pb[:, k * 128:(k + 1) * 128],
                                                    in_=xs[i][:, k * 128:(k + 1) * 128], identity=identb[:]),
                   [("xs", i), "identb"], [pbk])
            OP("act", lambda e: e.activation(out=xsT[i][:].rearrange("p k t -> p (k t)"), in_=pb[:], func=AF.Copy),
               [pbk], [("xsT", i)])
            return xsT[i], ("xsT", i)

        def alloc_ple_w(es):
            b = {}
            b["Wg"] = sb(es, "Wg", [128, 8, 1024], BF16)
            b["Wp"] = sb(es, "Wp", [128, 2, 1024], BF16)
            return b

        def alloc_common(es, b):
            b["ht"] = [sb(es, "ht%d" % i, [128, D], F32) for i in range(3)]
            b["junk"] = sb(es, "junk", [128, D], BF16)
            b["ss"] = sb(es, "ss", [128, 4], F32)
            b["rstd"] = sb(es, "rstd", [128, 4], F32)
            b["xs"] = [sb(es, "xs%d" % i, [128, D], BF16) for i in range(3)]
            b["xsT"] = [sb(es, "xsT%d" % i, [128, 8, 128], BF16) for i in range(3)]
            b["pt"] = [sb(es, "pt%d" % i, [128, 256], F32) for i in range(2)]
            b["pb16"] = sb(es, "pb16", [128, 256], BF16)
            b["pT"] = sb(es, "pT", [128, 2, 128], BF16)
            b["TP"] = sb(es, "TP", [128, D], F32)
            return b

        def load_h(bufs, hsrc, t):
            hs = t % 3
            S.dma("sp", ("ht", hs),
                  [lambda e: e.dma_start(out=bufs["ht"][hs][:], in_=hsrc[t * 128:(t + 1) * 128, :])],
                  writes=[("ht", hs)])

        def load_p(bufs, l, t):
            s2 = t % 2
            S.dma("sp", ("pt", s2),
                  [lambda e: e.dma_start(out=bufs["pt"][s2][:], in_=p_d[l, t * 128:(t + 1) * 128, :])],
                  writes=[("pt", s2)])

        def load_tile(bufs, l, hsrc, t):
            hs = t % 3
            S.dma("sp", ("ht", hs),
                  [lambda e: e.dma_start(out=bufs["ht"][hs][:], in_=hsrc[t * 128:(t + 1) * 128, :])],
                  writes=[("ht", hs)])
            s2 = t % 2
            S.dma("sp", ("pt", s2),
                  [lambda e: e.dma_start(out=bufs["pt"][s2][:], in_=p_d[l, t * 128:(t + 1) * 128, :])],
                  writes=[("pt", s2)])

        def ple_and_store(bufs, l, t, hdst):
            hs = t % 3
            X = bufs["ht"][hs]
            hk = ("ht", hs)
            s2 = t % 2
            xT2, xT2k = norm_T(bufs, X[:], hk, slot=2)
            pb16, pT, TP = bufs["pb16"], bufs["pT"], bufs["TP"]
            OP("dve", lambda e: e.tensor_copy(out=pb16[:], in_=bufs["pt"][s2][:]), [("pt", s2)], ["pb16"])
            pb, pbk = nextb()
            for k in range(2):
                OP("pe", lambda e, k=k: e.transpose(out=pb[:, k * 128:(k + 1) * 128],
                                                    in_=pb16[:, k * 128:(k + 1) * 128], identity=identb[:]),
                   ["pb16", "identb"], [pbk])
            OP("dve", lambda e: e.tensor_copy(out=pT[:].rearrange("p k t -> p (k t)"), in_=pb[:, 0:256]),
               [pbk], ["pT"])
            Wg, Wp = bufs["Wg"], bufs["Wp"]
            for nb in range(2):
                sl = slice(nb * 512, (nb + 1) * 512)
                pg, pgk = nextf()
                for k in range(8):
                    mm(pg[:], xT2[:, k, :], Wg[:, k, sl], k == 0, k == 7, [xT2k] + wk("Wg"), [pgk])
                pp, ppk = nextf()
                for k in range(2):
                    mm(pp[:], pT[:, k, :], Wp[:, k, sl], k == 0, k == 1, ["pT"] + wk("Wp"), [ppk])
                tk = ("TP", nb)
                sigmoid_inplace(TP[:, sl], pg[:], [pgk], tk)
                OP("dve", lambda e, sl=sl, pp=pp: e.tensor_tensor(out=TP[:, sl], in0=pp[:], in1=TP[:, sl],
                                                                 op=ALU.mult), [ppk, tk], [tk])
                OP("dve", lambda e, sl=sl: e.tensor_tensor(out=X[:, sl], in0=X[:, sl], in1=TP[:, sl], op=ALU.add),
                   [hk, tk], [hk])
            return S.dma("pool", ("st", hs),
                         [lambda e: e.dma_start(out=hdst[t * 128:(t + 1) * 128, :], in_=X[:])], reads=[hk])

        def layer_A(l, hsrc, hdst):
            S.new_epoch()
            S.barrier()
            es = contextlib.ExitStack()
            with es:
                bufs = alloc_ple_w(es)
                Win = sb(es, "Win", [128, 8, 4104], BF16)
                Wout = sb(es, "Wout", [128, 8, 1024], BF16)
                load_weights([(Win, w_in_a_d[l], 8, 4104, CV[("norm_a", l)], "Win"),
                              (Wout, w_out_a_d[l], 8, 1024, CV[("hnorm_a", l)], "Wout"),
                              (bufs["Wg"], w_pg_d[l], 8, 1024, CV[("ple_norm", l)], "Wg"),
                              (bufs["Wp"], w_pp_d[l], 2, 1024, None, "Wp")], nstg=6)
                alloc_common(es, bufs)
                xq = sb(es, "xq", [128, 8, 131], F32)
                T0 = sb(es, "T0", [128, D], F32)
                T1 = sb(es, "T1", [128, D], F32)
                T2 = [sb(es, "T2_%d" % i, [128, D], BF16) for i in range(2)]
                T3 = [sb(es, "T3_%d" % i, [128, D], BF16) for i in range(2)]
                T3sb = [sb(es, "T3s%d" % i, [128, 512], F32) for i in range(2)]
                qkT = [sb(es, "qkT%d" % i, [128, 8, 128], BF16) for i in range(2)]
                Ktok = [sb(es, "Ktok%d" % i, [128, 512], BF16) for i in range(2)]
                uV = [sb(es, "uV%d" % i, [128, 4, 258], BF16) for i in range(2)]
                PT = sb(es, "PT", [128, 4, 128], BF16)
                ho = sb(es, "ho", [128, D], F32)
                yb = sb(es, "yb", [128, D], BF16)
                yT = sb(es, "yT", [128, 8, 128], BF16)
                Dst = sb(es, "Dst", [128, 4, 257], F32)
                Cb = sb(es, "Cb", [128, 4, 258], BF16)
                sm = sb(es, "sm", [128, 96], F32)
                g8 = sm[:, 0:8]; l4 = sm[:, 8:12]; iB = sm[:, 16:20]
                u4 = sm[:, 20:24]; t1 = sm[:, 24:28]; r4 = sm[:, 28:32]; ssh = sm[:, 32:36]
                rsh = sm[:, 36:40]
                E3c = [40, 44, 48]
                eBc = [52, 56]
                T0v = T0[:].rearrange("p (c t) -> p c t", c=8)
                T1v = T1[:].rearrange("p (c t) -> p c t", c=8)
                OP("pool", lambda e: e.memset(xq[:, :, 0:3], 0.0), [], [("xq", 0), ("xq", 1)])
                cb = CV[("conv_a", l)]
                bg = RW[("b_gate", l)]
                ctx = {}

                def s0(t):
                    hs = t % 3
                    X = bufs["ht"][hs]
                    ctx[t] = norm_T(bufs, X[:], ("ht", hs), slot=t % 2)

                def s1(t):
                    i2 = t % 2
                    xT, xTk = ctx.pop(t)
                    eBo = eBc[i2]
                    Eo = E3c[t % 3]
                    eB = sm[:, eBo:eBo + 4]
                    Ecur = sm[:, Eo:Eo + 4]
                    pfg, pfgk = nextf()
                    for k in range(8):
                        mm(pfg[:, 0:8], xT[:, k, :], Win[:, k, 4096:4104], k == 0, k == 7, [xTk] + wk("Win"), [pfgk])
                    OP("dve", lambda e: e.tensor_tensor(out=g8, in0=pfg[:, 0:8], in1=rows[:, bg:bg + 8],
                                                        op=ALU.add), [pfgk, "rows"], ["g8"])
                    OP("act", lambda e: e.activation(out=l4, in_=sm[:, 4:8], func=AF.Exp, scale=-1.0), ["g8"], ["l4"])
                    OP("act", lambda e: e.activation(out=l4, in_=l4, func=AF.Ln, bias=1.0), ["l4"], ["l4"])
                    mm(pfg[:, 8:12], triuf, l4, True, True, ["cstf", "l4"], [pfgk])
                    mm(pfg[:, 12:16], onesf, l4, True, True, ["cstf", "l4"], [pfgk])
                    OP("act", lambda e: e.activation(out=eB, in_=pfg[:, 8:12], func=AF.Exp, scale=-1.0),
                       [pfgk], [("eB", i2)])
                    OP("dve", lambda e: e.tensor_tensor(out=iB, in0=sm[:, 0:4], in1=pfg[:, 8:12], op=ALU.add),
                       [pfgk, "g8"], ["iB"])
                    OP("act", lambda e: e.activation(out=u4, in_=iB, func=AF.Exp, bias=LNSK), ["iB"], ["u4"])
                    OP("act", lambda e: e.activation(out=Ecur, in_=pfg[:, 12:16], func=AF.Exp, scale=-1.0),
                       [pfgk], [("E", t % 3)])
                    if t > 0:
                        OP("dve", lambda e: e.tensor_copy(out=xq[:, :, 0:3], in_=xq[:, :, 128:131]),
                           [("xq", 0), ("xq", 1)], [("xq", 0), ("xq", 1)])
                    for half in range(2):
                        pf, pfk = nextf()
                        for c4 in range(4):
                            c = half * 4 + c4
                            for k in range(8):
                                mm(pf[:, c4 * 128:(c4 + 1) * 128], Win[:, k, c * 128:(c + 1) * 128], xT[:, k, :],
                                   k == 0, k == 7, [xTk] + wk("Win"), [pfk])
                        OP("act", lambda e, pf=pf, half=half: e.activation(
                            out=xq[:, half * 4:half * 4 + 4, 3:131],
                            in_=pf[:].rearrange("p (c t) -> p c t", c=4), func=AF.Copy), [pfk], [("xq", half)])

                    def wc(j):
                        return cvec[:, cb + j * 8:cb + j * 8 + 8].unsqueeze(2).to_broadcast([128, 8, 128])
                    xqk = [("xq", 0), ("xq", 1)]
                    OP("dve", lambda e: e.tensor_tensor(out=T0v, in0=xq[:, :, 3:131], in1=wc(3), op=ALU.mult),
                       xqk + ["cvec"], ["T0"])
                    for j in (2, 1, 0):
                        OP("dve", lambda e, j=j: e.tensor_tensor(out=T1v, in0=xq[:, :, j:j + 128], in1=wc(j),
                                                                op=ALU.mult), xqk + ["cvec"], ["T1"])
                        OP("dve", lambda e: e.tensor_tensor(out=T0[:], in0=T0[:], in1=T1[:], op=ALU.add),
                           ["T0", "T1"], ["T0"])
                    sigmoid_inplace(T1[:], T0[:], ["T0"], "T1")
                    qk = qkT[i2]
                    qkk = ("qkT", i2)
                    OP("dve", lambda e: e.tensor_tensor(out=qk[:].rearrange("p c t -> p (c t)"), in0=T0[:],
                                                        in1=T1[:], op=ALU.mult), ["T0", "T1"], [qkk])
                    pb, pbk = nextb()
                    for h in range(4):
                        OP("pe", lambda e, h=h: e.transpose(out=pb[:, h * 128:(h + 1) * 128],
                                                            in_=qk[:, 4 + h, :], identity=identb[:]),
                           [qkk, "identb"], [pbk])
                    Kt = Ktok[i2]
                    Ktk = ("Ktok", i2)
                    OP("dve", lambda e: e.tensor_copy(out=Kt[:], in_=pb[:, 0:512]), [pbk], [Ktk])
                    uVc = uV[i2]
                    uVk = ("uV", i2)
                    T2c = T2[i2]
                    T3c = T3[i2]
                    for nb in range(6):
                        pf, pfk = nextf()
                        for k in range(8):
                            mm(pf[:], xT[:, k, :], Win[:, k, 1024 + nb * 512:1024 + (nb + 1) * 512],
                               k == 0, k == 7, [xTk] + wk("Win"), [pfk])
                        if nb < 2:
                            for hh in range(2):
                                h = 2 * nb + hh
                                OP("act", lambda e, h=h, hh=hh, pf=pf: e.activation(
                                    out=uVc[:, h, 0:256], in_=pf[:, hh * 256:(hh + 1) * 256], func=AF.Copy,
                                    scale=sm[:, 20 + h:21 + h]), [pfk, "u4"], [uVk])
                        elif nb < 4:
                            sl = slice((nb - 2) * 512, (nb - 1) * 512)
                            T3s = T3sb[nb % 2]; tsk = ("T3s", nb % 2)
                            sigmoid_inplace(T3s[:], pf[:], [pfk], tsk)
                            OP("dve", lambda e, sl=sl, T3s=T3s: e.tensor_copy(out=T2c[:, sl], in_=T3s[:]),
                               [tsk], [("T2", i2, nb - 2)])
                        else:
                            sl = slice((nb - 4) * 512, (nb - 3) * 512)
                            T3s = T3sb[nb % 2]; tsk = ("T3s", nb % 2)
                            sigmoid_inplace(T3s[:], pf[:], [pfk], tsk)
                            OP("dve", lambda e, sl=sl, pf=pf, T3s=T3s: e.tensor_tensor(out=T3c[:, sl], in0=pf[:],
                                                                                      in1=T3s[:], op=ALU.mult),
                               [pfk, tsk], [("T3", i2, nb - 4)])
                    OP("dve", lambda e: e.tensor_copy(out=uVc[:, :, 256], in_=u4), ["u4"], [uVk])

                def s2(t):
                    i2 = t % 2
                    eBo = eBc[i2]
                    eB = sm[:, eBo:eBo + 4]
                    Eo = E3c[t % 3]
                    Ecur = sm[:, Eo:Eo + 4]
                    Epo = E3c[(t - 1) % 3]
                    qk = qkT[i2]; qkk = ("qkT", i2)
                    Kt = Ktok[i2]; Ktk = ("Ktok", i2)
                    uVc = uV[i2]; uVk = ("uV", i2)
                    T2c = T2[i2]; T3c = T3[i2]
                    pS, pSk = nextf()
                    for h in range(4):
                        mm(pS[:, h * 128:(h + 1) * 128], qk[:, 4 + h, :], qk[:, h, :], True, True, [qkk], [pSk])
                    OP("dve", lambda e: e.tensor_tensor(
                        out=PT[:], in0=pS[:].rearrange("p (h t) -> p h t", h=4),
                        in1=triub[:].unsqueeze(1).to_broadcast([128, 4, 128]), op=ALU.mult), [pSk, "triub"], ["PT"])
                    pas = []
                    for h in range(4):
                        pa, pak = nextf()
                        pas.append((pa, pak))
                        mm(pa[:, 0:257], PT[:, h, :], uVc[:, h, 0:257], True, t == 0, ["PT", uVk], [pak])
                        if t > 0:
                            mm(pa[:, 0:257], qk[:, h, :], Cb[:, h, 0:257], False, True, [qkk, "Cb"], [pak])
                        OP("dve", lambda e, h=h, pa=pa: e.tensor_tensor(out=sm[:, 24 + h:25 + h],
                                                                       in0=sm[:, eBo + h:eBo + h + 1],
                                                                       in1=pa[:, 256:257], op=ALU.mult),
                           [pak, ("eB", i2)], [("t1", h)])
                    pus = []
                    for h in range(2):
                        pu, puk = nextf()
                        pus.append((pu, puk))
                        mm(pu[:, 0:257], Kt[:, h * 128:(h + 1) * 128], uVc[:, h, 0:257], True, True, [Ktk, uVk], [puk])
                    t1k = [("t1", h) for h in range(4)]
                    OP("act", lambda e: e.activation(out=t1, in_=t1, func=AF.Abs), t1k, t1k)
                    OP("dve", lambda e: e.tensor_scalar(out=t1, in0=t1, scalar1=1.0, scalar2=None, op0=ALU.max),
                       t1k, t1k)
                    OP("dve", lambda e: e.reciprocal(out=t1, in_=t1), t1k, t1k)
                    OP("dve", lambda e: e.tensor_tensor(out=r4, in0=eB, in1=t1, op=ALU.mult),
                       t1k + [("eB", i2)], ["r4"])
                    for h in range(4):
                        pa, pak = pas[h]
                        hsl = slice(h * 256, (h + 1) * 256)
                        OP("dve", lambda e, h=h, pa=pa, hsl=hsl: e.scalar_tensor_tensor(
                            out=ho[:, hsl], in0=pa[:, 0:256], scalar=sm[:, 28 + h:29 + h], in1=T2c[:, hsl],
                            op0=ALU.mult, op1=ALU.mult), [pak, "r4", ("T2", i2, h // 2)], [("ho", h)])
                        OP("act", lambda e, h=h, hsl=hsl: e.activation(out=bufs["junk"][:, 0:256], in_=ho[:, hsl],
                                                                      func=AF.Square, accum_out=sm[:, 32 + h:33 + h]),
                           [("ho", h)], ["junk", ("ssh", h)])
                    for h in range(2, 4):
                        pu, puk = nextf()
                        pus.append((pu, puk))
                        mm(pu[:, 0:257], Kt[:, h * 128:(h + 1) * 128], uVc[:, h, 0:257], True, True, [Ktk, uVk], [puk])
                    for h in range(4):
                        pu, puk = pus[h]
                        if t == 0:
                            OP("dve", lambda e, h=h, pu=pu: e.tensor_copy(out=Dst[:, h, :], in_=pu[:, 0:257]),
                               [puk], [("D", h)])
                        else:
                            OP("dve", lambda e, h=h, pu=pu: e.scalar_tensor_tensor(
                                out=Dst[:, h, :], in0=Dst[:, h, :], scalar=sm[:, Epo + h:Epo + h + 1], in1=pu[:, 0:257],
                                op0=ALU.mult, op1=ALU.add), [puk, ("D", h), ("E", (t - 1) % 3)], [("D", h)])
                        OP("act", lambda e, h=h: e.activation(out=Cb[:, h, 0:257], in_=Dst[:, h, :],
                                                              func=AF.Copy, scale=sm[:, Eo + h:Eo + h + 1]),
                           [("D", h), ("E", t % 3)], ["Cb"])
                    sshk = [("ssh", h) for h in range(4)]
                    OP("act", lambda e: e.activation(out=rsh, in_=ssh, func=AF.Ln, scale=1.0 / 256, bias=EPS),
                       sshk, ["rsh"])
                    OP("act", lambda e: e.activation(out=rsh, in_=rsh, func=AF.Exp, scale=-0.5), ["rsh"], ["rsh"])
                    for h in range(4):
                        hsl = slice(h * 256, (h + 1) * 256)
                        OP("dve", lambda e, h=h, hsl=hsl: e.scalar_tensor_tensor(
                            out=yb[:, hsl], in0=ho[:, hsl], scalar=sm[:, 36 + h:37 + h], in1=T3c[:, hsl],
                            op0=ALU.mult, op1=ALU.mult), [("ho", h), "rsh", ("T3", i2, h // 2)], [("yb", h)])

                def s3(t):
                    hs = t % 3
                    X = bufs["ht"][hs]
                    hk = ("ht", hs)
                    pb, pbk = nextb()
                    for k in range(8):
                        OP("pe", lambda e, k=k: e.transpose(out=pb[:, k * 128:(k + 1) * 128],
                                                            in_=yb[:, k * 128:(k + 1) * 128], identity=identb[:]),
                           [("yb", k // 2), "identb"], [pbk])
                    OP("act", lambda e: e.activation(out=yT[:].rearrange("p k t -> p (k t)"), in_=pb[:],
                                                     func=AF.Copy), [pbk], ["yT"])
                    for nb in range(2):
                        sl = slice(nb * 512, (nb + 1) * 512)
                        pf, pfk = nextf()
                        for k in range(8):
                            mm(pf[:], yT[:, k, :], Wout[:, k, sl], k == 0, k == 7, ["yT"] + wk("Wout"), [pfk])
                        OP("dve", lambda e, sl=sl, pf=pf: e.tensor_tensor(out=X[:, sl], in0=X[:, sl], in1=pf[:],
                                                                         op=ALU.add), [pfk, hk], [hk])
                    ple_and_store(bufs, l, t, hdst)

                load_h(bufs, hsrc, 0)
                if ntiles > 1:
                    load_h(bufs, hsrc, 1)
                load_p(bufs, l, 0)
                s0(0)
                s1(0)
                if ntiles > 1:
                    s0(1)
                for t in range(ntiles):
                    if t + 2 < ntiles:
                        load_h(bufs, hsrc, t + 2)
                    if t + 1 < ntiles:
                        load_p(bufs, l, t + 1)
                        s1(t + 1)
                    s2(t)
                    if t + 2 < ntiles:
                        s0(t + 2)
                    s3(t)
                S.barrier()

        SCALE = 128.0 ** -0.5
        AX = mybir.AxisListType.X

        def headnorm_rope(tb, pf, pfk, nrm_col, t, outv, outk):
            tid = tb.get("id", 0)
            sq, qf, ssq, rsq, ta, tbb, cosT, sinT = (tb["sq"], tb["qf"], tb["ssq"], tb["rsq"], tb["ta"], tb["tb"],
                                                     tb["cos"], tb["sin"])
            ksq, kqf, kss, krs, kta, ktb = [(n, tid) for n in ("sq", "qf", "ssq", "rsq", "ta", "tb")]
            pv = pf[:].rearrange("p (h d) -> p h d", h=4)
            qv = qf[:].rearrange("p (h d) -> p h d", h=4)
            OP("act", lambda e: e.activation(out=sq[:], in_=pf[:], func=AF.Square), [pfk], [ksq])
            OP("dve", lambda e: e.tensor_reduce(out=ssq[:], in_=sq[:].rearrange("p (h d) -> p h d", h=4), axis=AX,
                                                op=ALU.add), [ksq], [kss])
            OP("act", lambda e: e.activation(out=rsq[:], in_=ssq[:], func=AF.Ln, scale=1.0 / 128, bias=EPS),
               [kss], [krs])
            OP("act", lambda e: e.activation(out=rsq[:], in_=rsq[:], func=AF.Exp, scale=-0.5), [krs], [krs])
            OP("dve", lambda e: e.tensor_tensor(out=qv, in0=pv, in1=rsq[:].unsqueeze(2).to_broadcast([128, 4, 128]),
                                                op=ALU.mult), [pfk, krs], [kqf])
            OP("dve", lambda e: e.tensor_tensor(
                out=qv, in0=qv, in1=rows[:, nrm_col:nrm_col + 128].unsqueeze(1).to_broadcast([128, 4, 128]),
                op=ALU.mult), [kqf, "rows"], [kqf])
            cb_ = cosT[:, t, :].unsqueeze(1).to_broadcast([128, 4, 64])
            sb_ = sinT[:, t, :].unsqueeze(1).to_broadcast([128, 4, 64])
            x1 = qv[:, :, 0:64]
            x2 = qv[:, :, 64:128]
            OP("dve", lambda e: e.tensor_tensor(out=ta[:], in0=x1, in1=cb_, op=ALU.mult), [kqf, "cs"], [kta])
            OP("dve", lambda e: e.tensor_tensor(out=tbb[:], in0=x2, in1=sb_, op=ALU.mult), [kqf, "cs"], [ktb])
            OP("dve", lambda e: e.tensor_tensor(out=outv[:, :, 0:64], in0=ta[:], in1=tbb[:], op=ALU.subtract),
               [kta, ktb], [outk])
            OP("dve", lambda e: e.tensor_tensor(out=ta[:], in0=x2, in1=cb_, op=ALU.mult), [kqf, "cs"], [kta])
            OP("dve", lambda e: e.tensor_tensor(out=tbb[:], in0=x1, in1=sb_, op=ALU.mult), [kqf, "cs"], [ktb])
            OP("dve", lambda e: e.tensor_tensor(out=outv[:, :, 64:128], in0=ta[:], in1=tbb[:], op=ALU.add),
               [kta, ktb], [outk])

        def phase_B1(l, j, hsrc, KT, VT):
            S.new_epoch()
            S.barrier()
            es = contextlib.ExitStack()
            with es:
                bufs = {}
                Wq = sb(es, "Wq", [128, 8, 1536], BF16)
                specs = [(Wq, w_in_b_d[j][:, :, 0:1536], 8, 1536, CV[("norm_b", j)], "Wq")]
                if j == 0:
                    Wkv = sb(es, "Wkv", [128, 8, 1024], BF16)
                    specs.append((Wkv, w_kv_d, 8, 1024, CV["norm_kv"], "Wkv"))
                load_weights(specs, nstg=5)
                bufs["ht"] = [sb(es, "ht%d" % i, [128, D], F32) for i in range(3)]
                bufs["junk"] = sb(es, "junk", [128, D], BF16)
                bufs["ss"] = sb(es, "ss", [128, 2], F32)
                bufs["rstd"] = sb(es, "rstd", [128, 2], F32)
                bufs["xs"] = [sb(es, "xs%d" % i, [128, D], BF16) for i in range(2)]
                bufs["xsT"] = [sb(es, "xsT%d" % i, [128, 8, 128], BF16) for i in range(2)]
                cosT = sb(es, "cosT", [128, NT, 64], F32)
                sinT = sb(es, "sinT", [128, NT, 64], F32)
                tbs = []
                for i in range(2):
                    tb = {"id": i, "cos": cosT, "sin": sinT}
                    tb["sq"] = sb(es, "sq", [128, 512], F32)
                    tb["qf"] = sb(es, "qf", [128, 512], F32)
                    tb["ssq"] = sb(es, "ssq", [128, 4], F32)
                    tb["rsq"] = sb(es, "rsq", [128, 4], F32)
                    tb["ta"] = sb(es, "ta", [128, 4, 64], F32)
                    tb["tb"] = sb(es, "tb", [128, 4, 64], F32)
                    tbs.append(tb)
                qr = [sb(es, "qr%d" % i, [128, 12, 128], BF16) for i in range(2)]
                kr = [sb(es, "kr%d" % i, [128, 4, 128], BF16) for i in range(2)]
                qTs = [sb(es, "qTs%d" % i, [128, 12, 128], BF16) for i in range(2)]
                S.dma("sp", "cs", [lambda e: e.dma_start(out=cosT[:], in_=cos_d),
                                   lambda e: e.dma_start(out=sinT[:], in_=sin_d)], writes=["cs"])

                def ld(t):
                    hs = t % 3
                    S.dma("sp", ("ht", hs),
                          [lambda e: e.dma_start(out=bufs["ht"][hs][:], in_=hsrc[t * 128:(t + 1) * 128, :])],
                          writes=[("ht", hs)])
                qn = RW[("qnorm", j)]
                ctx = {}
                chain = [0]

                def Sa(t):
                    hs = t % 3
                    ctx[("x", t)] = norm_T(bufs, bufs["ht"][hs][:], ("ht", hs), slot=t % 2)

                def Sb(t):
                    xT, xTk = ctx.pop(("x", t))
                    banks = {}
                    if j == 0:
                        pf, pfk = nextf()
                        for c in range(4):
                            for k in range(8):
                                mm(pf[:, c * 128:(c + 1) * 128], Wkv[:, k, 512 + c * 128:512 + (c + 1) * 128],
                                   xT[:, k, :], k == 0, k == 7, [xTk] + wk("Wkv"), [pfk])
                        banks["v"] = (pf, pfk)
                        pf, pfk = nextf()
                        for k in range(8):
                            mm(pf[:], xT[:, k, :], Wkv[:, k, 0:512], k == 0, k == 7, [xTk] + wk("Wkv"), [pfk])
                        banks["k"] = (pf, pfk)
                    for g in range(3):
                        pf, pfk = nextf()
                        for k in range(8):
                            mm(pf[:], xT[:, k, :], Wq[:, k, g * 512:(g + 1) * 512], k == 0, k == 7,
                               [xTk] + wk("Wq"), [pfk])
                        banks[g] = (pf, pfk)
                    ctx[("b", t)] = banks

                def hr(pf, pfk, col, t, outv, outk):
                    tb = tbs[chain[0] % 2]; chain[0] += 1
                    headnorm_rope(tb, pf, pfk, col, t, outv, outk)

                def Sc(t):
                    banks = ctx.pop(("b", t))
                    tsl = slice(t * 128, (t + 1) * 128)
                    i2 = t % 2
                    if j == 0:
                        pfv, pfvk = banks["v"]
                        OP("act", lambda e: e.activation(
                            out=VT[:, :, tsl], in_=pfv[:].rearrange("p (c t) -> p c t", c=4), func=AF.Copy),
                           [pfvk], ["VT"])
                        pf, pfk = banks["k"]
                        hr(pf, pfk, RW["knorm"], t, kr[i2][:], ("kr", i2))
                    for g in range(3):
                        pf, pfk = banks[g]
                        hr(pf, pfk, qn, t, qr[i2][:, g * 4:(g + 1) * 4, :], ("qr", i2, g))

                def Sd(t):
                    tsl = slice(t * 128, (t + 1) * 128)
                    i2 = t % 2
                    if j == 0:
                        pb, pbk = nextb()
                        for h in range(4):
                            OP("pe", lambda e, h=h: e.transpose(out=pb[:, h * 128:(h + 1) * 128], in_=kr[i2][:, h, :],
                                                                identity=identb[:]), [("kr", i2), "identb"], [pbk])
                        OP("act", lambda e: e.activation(
                            out=KT[:, :, tsl], in_=pb[:, 0:512].rearrange("p (c t) -> p c t", c=4), func=AF.Copy),
                           [pbk], ["KT"])
                    qs = qTs[i2]
                    qsk = ("qTs", i2)
                    for half in range(2):
                        nh = 8 if half == 0 else 4
                        pb2, pb2k = nextb()
                        for hh in range(nh):
                            gh = half * 8 + hh
                            OP("pe", lambda e, hh=hh, gh=gh, pb2=pb2: e.transpose(
                                out=pb2[:, hh * 128:(hh + 1) * 128], in_=qr[i2][:, gh, :], identity=identb[:]),
                               [("qr", i2, gh // 4), "identb"], [pb2k])
                        OP("act", lambda e, pb2=pb2, half=half, nh=nh: e.activation(
                            out=qs[:, half * 8:half * 8 + nh, :],
                            in_=pb2[:, 0:nh * 128].rearrange("p (c t) -> p c t", c=nh), func=AF.Copy),
                           [pb2k], [(qsk, half)])
                    S.dma("pool", ("qst", i2),
                          [lambda e: e.dma_start(
                              out=qT_d[:, :, :, tsl].rearrange("h d g t -> d g h t"),
                              in_=qs[:].rearrange("p (g h) t -> p g h t", g=3))],
                          reads=[(qsk, 0), (qsk, 1)], writes=["qT_d"])

                for t in range(min(3, ntiles)):
                    ld(t)
                Sa(0)
                if ntiles > 1:
                    Sa(1)
                Sb(0)
                for t in range(ntiles):
                    Sc(t)
                    if t + 3 < ntiles:
                        ld(t + 3)
                    if t + 2 < ntiles:
                        Sa(t + 2)
                    if t + 1 < ntiles:
                        Sb(t + 1)
                    Sd(t)
                S.barrier()

        def phase_B2(KT, VT, oT):
            S.new_epoch()
            S.barrier()
            es = contextlib.ExitStack()
            with es:
                QTh = sb(es, "QTh", [128, 3, S_LEN], BF16)
                Vg = [sb(es, "Vg%d" % g, [128, 32, 128], BF16) for g in range(3)]
                num = sb(es, "num", [128, S_LEN], F32)
                den = sb(es, "den", [128, S_LEN], F32)
                PTa = [sb(es, "PTa%d" % i, [128, 4, 128], BF16) for i in range(4)]
                pc = [0]
                for h in range(4):
                    S.dma("sp", "qld", [lambda e, h=h: e.dma_start(out=QTh[:], in_=qT_d[h])],
                          reads=["qT_d"], writes=["QTh"])
                    for g, r in enumerate((1, 4, 16)):
                        nbk = 32 // r
                        for b0 in range(0, 32, 8):
                            pb, pbk = nextb()
                            for bb in range(8):
                                blk = b0 + bb
                                c, n = blk // nbk, blk % nbk
                                base = c + r * 128 * n
                                OP("pe", lambda e, pb=pb, bb=bb, h=h, base=base, r=r: e.transpose(
                                    out=pb[:, bb * 128:(bb + 1) * 128],
                                    in_=VT[:, h, base:base + r * 127 + 1:r], identity=identb[:]),
                                   ["VT", "identb"], [pbk])
                            OP("act", lambda e, pb=pb, g=g, b0=b0: e.activation(
                                out=Vg[g][:, b0:b0 + 8, :], in_=pb[:].rearrange("p (c t) -> p c t", c=8),
                                func=AF.Copy), [pbk], [("Vg", g)])
                    items = []
                    for g, r in enumerate((1, 4, 16)):
                        if g == 0:
                            for m in range(8):
                                items.append((g, r, [(0, 4 * m + jb) for jb in range(4)],
                                              lambda a, m=m: a[:, 512 * m:512 * (m + 1)].rearrange(
                                                  "p (j i) -> p j i", j=4)))
                        elif g == 1:
                            for n in range(8):
                                items.append((g, r, [(c, n) for c in range(4)],
                                              lambda a, n=n: a[:, 512 * n:512 * (n + 1)].rearrange(
                                                  "p (i c) -> p c i", c=4)))
                        else:
                            for n in range(2):
                                for m in range(4):
                                    items.append((g, r, [(4 * m + jb, n) for jb in range(4)],
                                                  lambda a, n=n, m=m: a[:, 2048 * n:2048 * (n + 1)].rearrange(
                                                      "p (i c) -> p c i", c=16)[:, 4 * m:4 * m + 4, :]))

                    def P(item):
                        g, r, blocks, view = item
                        pts = []
                        for half in range(2):
                            ps_, psk = nextf()
                            pt_ = PTa[pc[0] % 4]
                            ptk = ("PTa", pc[0] % 4)
                            pc[0] += 1
                            for q2 in range(2):
                                c, n = blocks[half * 2 + q2]
                                b_ = c + r * 128 * n
                                qsl = slice(b_, b_ + r * 127 + 1, r)
                                mm(ps_[:, q2 * 256:q2 * 256 + 128], KT[:, h, qsl], QTh[:, g, qsl], True, True,
                                   ["KT", "QTh"], [psk])
                                if n >= 1:
                                    bp = c + r * 128 * (n - 1)
                                    psl = slice(bp, bp + r * 127 + 1, r)
                                    mm(ps_[:, q2 * 256 + 128:q2 * 256 + 256], KT[:, h, psl], QTh[:, g, qsl],
                                       True, True, ["KT", "QTh"], [psk])
                            OP("act", lambda e, ps_=ps_, pt_=pt_: e.activation(
                                out=pt_[:].rearrange("p a b -> p (a b)"), in_=ps_[:], func=AF.Exp, scale=SCALE),
                               [psk], [ptk])
                            OP("dve", lambda e, pt_=pt_: e.tensor_tensor(out=pt_[:], in0=pt_[:], in1=mask2[:],
                                                                        op=ALU.mult), [ptk, "mask2"], [ptk])
                            pts.append((pt_, ptk))
                        return pts

                    def Q(item, pts):
                        g, r, blocks, view = item
                        nbk = 32 // r
                        pO, pOk = nextf()
                        pD, pDk = nextf()
                        for jb in range(4):
                            c, n = blocks[jb]
                            pt_, ptk = pts[jb // 2]
                            q2 = jb % 2
                            bi = c * nbk + n
                            osl = slice(jb * 128, (jb + 1) * 128)
                            mm(pO[:, osl], Vg[g][:, bi, :], pt_[:, q2 * 2, :], True, n == 0,
                               [("Vg", g), ptk], [pOk])
                            if n >= 1:
                                mm(pO[:, osl], Vg[g][:, bi - 1, :], pt_[:, q2 * 2 + 1, :], False, True,
                                   [("Vg", g), ptk], [pOk])
                            mm(pD[:, osl], onesb[:], pt_[:, q2 * 2, :], True, n == 0, ["onesb", ptk], [pDk])
                            if n >= 1:
                                mm(pD[:, osl], onesb[:], pt_[:, q2 * 2 + 1, :], False, True, ["onesb", ptk], [pDk])
                        nv = view(num)
                        dv = view(den)
                        pOv = pO[:].rearrange("p (j i) -> p j i", j=4)
                        pDv = pD[:].rearrange("p (j i) -> p j i", j=4)
                        if g == 0:
                            OP("act", lambda e: e.activation(out=nv, in_=pOv, func=AF.Copy), [pOk], ["num"])
                            OP("dve", lambda e: e.tensor_copy(out=dv, in_=pDv), [pDk], ["den"])
                        else:
                            OP("dve", lambda e: e.tensor_tensor(out=nv, in0=nv, in1=pOv, op=ALU.add),
                               [pOk, "num"], ["num"])
                            OP("dve", lambda e: e.tensor_tensor(out=dv, in0=dv, in1=pDv, op=ALU.add),
                               [pDk, "den"], ["den"])

                    cur = P(items[0])
                    for bi_ in range(len(items)):
                        nxt = P(items[bi_ + 1]) if bi_ + 1 < len(items) else None
                        Q(items[bi_], cur)
                        cur = nxt
                    OP("act", lambda e: e.activation(out=den[:], in_=den[:], func=AF.Ln), ["den"], ["den"])
                    OP("act", lambda e: e.activation(out=den[:], in_=den[:], func=AF.Exp, scale=-1.0), ["den"], ["den"])
                    OP("dve", lambda e, h=h: e.tensor_tensor(out=oT[:, h, :], in0=num[:], in1=den[:], op=ALU.mult),
                       ["num", "den"], ["oT"])
                S.barrier()

        def phase_B3(l, j, hsrc, hdst, oT):
            S.new_epoch()
            S.barrier()
            es = contextlib.ExitStack()
            with es:
                bufs = alloc_ple_w(es)
                Wz = sb(es, "Wz", [128, 8, 512], BF16)
                Wob = sb(es, "Wob", [128, 4, 1024], BF16)
                load_weights([(Wz, w_in_b_d[j][:, :, 1536:2048], 8, 512, CV[("norm_b", j)], "Wz"),
                              (Wob, w_out_b_d[j], 4, 1024, None, "Wob"),
                              (bufs["Wg"], w_pg_d[l], 8, 1024, CV[("ple_norm", l)], "Wg"),
                              (bufs["Wp"], w_pp_d[l], 2, 1024, None, "Wp")], nstg=3)
                alloc_common(es, bufs)
                Tz = sb(es, "Tz", [128, 512], F32)
                yTs = [sb(es, "yTb%d" % i, [128, 4, 128], BF16) for i in range(2)]
                ctx = {}

                def s0(t):
                    hs = t % 3
                    ctx[t] = norm_T(bufs, bufs["ht"][hs][:], ("ht", hs), slot=t % 2)

                def s1(t):
                    xT, xTk = ctx.pop(t)
                    tsl = slice(t * 128, (t + 1) * 128)
                    yT = yTs[t % 2]
                    pf, pfk = nextf()
                    for c in range(4):
                        for k in range(8):
                            mm(pf[:, c * 128:(c + 1) * 128], Wz[:, k, c * 128:(c + 1) * 128], xT[:, k, :],
                               k == 0, k == 7, [xTk] + wk("Wz"), [pfk])
                    sigmoid_inplace(Tz[:], pf[:], [pfk], "Tz")
                    OP("dve", lambda e: e.tensor_tensor(out=Tz[:], in0=pf[:], in1=Tz[:], op=ALU.mult),
                       [pfk, "Tz"], ["Tz"])
                    OP("dve", lambda e: e.tensor_tensor(
                        out=yT[:], in0=Tz[:].rearrange("p (c t) -> p c t", c=4), in1=oT[:, :, tsl], op=ALU.mult),
                       ["Tz", "oT"], [("yTb", t % 2)])

                def s3(t):
                    hs = t % 3
                    X = bufs["ht"][hs]
                    hk = ("ht", hs)
                    yT = yTs[t % 2]
                    for nb in range(2):
                        sl = slice(nb * 512, (nb + 1) * 512)
                        pf2, pf2k = nextf()
                        for c in range(4):
                            mm(pf2[:], yT[:, c, :], Wob[:, c, sl], c == 0, c == 3,
                               [("yTb", t % 2)] + wk("Wob"), [pf2k])
                        OP("dve", lambda e, sl=sl, pf2=pf2: e.tensor_tensor(out=X[:, sl], in0=X[:, sl], in1=pf2[:],
                                                                           op=ALU.add), [pf2k, hk], [hk])
                    ple_and_store(bufs, l, t, hdst)

                load_h(bufs, hsrc, 0)
                if ntiles > 1:
                    load_h(bufs, hsrc, 1)
                load_p(bufs, l, 0)
                s0(0)
                s1(0)
                if ntiles > 1:
                    s0(1)
                for t in range(ntiles):
                    if t + 2 < ntiles:
                        load_h(bufs, hsrc, t + 2)
                    if t + 1 < ntiles:
                        load_p(bufs, l, t + 1)
                        s1(t + 1)
                    if t + 2 < ntiles:
                        s0(t + 2)
                    s3(t)
                S.barrier()

        srcs = [x_d, hA_d, hB_d, hA_d]
        dsts = [hA_d, hB_d, hA_d, out_d]
        dsts[n_layers - 1] = out_d
        kvs = contextlib.ExitStack()
        KT = VT = None
        for l in range(n_layers):
            if l < 2:
                layer_A(l, srcs[l], dsts[l])
            else:
                j = l - 2
                if j == 0:
                    KT = sb(kvs, "KT", [128, 4, S_LEN], BF16)
                    VT = sb(kvs, "VT", [128, 4, S_LEN], BF16)
                    if ntiles < NT:
                        OP("pool", lambda e: e.memset(KT[:], 0.0), [], ["KT"])
                        OP("pool", lambda e: e.memset(VT[:], 0.0), [], ["VT"])
                phase_B1(l, j, srcs[l], KT, VT)
                with contextlib.ExitStack() as os_:
                    oT = sb(os_, "oT", [128, 4, S_LEN], BF16)
                    phase_B2(KT, VT, oT)
                    phase_B3(l, j, srcs[l], dsts[l], oT)
        kvs.close()
        finals = [o for k, o in S.last.items() if isinstance(k, tuple) and k[0] == "dma" and
                  isinstance(k[1], tuple) and k[1][0] == "st"]
        S.finalize_and_emit(final_waits=finals)
    return nc, S


def _wl(w):
    w = np.asarray(w, np.float32)
    return np.ascontiguousarray(w.reshape(-1, 128, w.shape[-1]).transpose(1, 0, 2))


def host_inputs(inp):
    g = {}
    g["w_in_a"] = np.stack([_wl(inp["w_in_a"][l]) for l in range(2)])
    g["w_out_a"] = np.stack([_wl(inp["w_out_a"][l]) for l in range(2)])
    g["w_kv"] = _wl(inp["w_kv"])
    g["w_in_b"] = np.stack([_wl(inp["w_in_b"][l]) for l in range(2)])
    g["w_out_b"] = np.stack([_wl(inp["w_out_b"][l]) for l in range(2)])
    g["w_ple_gate"] = np.stack([_wl(inp["w_ple_gate"][l]) for l in range(4)])
    g["w_ple"] = np.stack([_wl(inp["w_ple"][l]) for l in range(4)])
    cv = np.zeros((128, NCV), np.float32)
    for l in range(2):
        c = CV[("norm_a", l)]; cv[:, c:c + 8] = _pk(inp["norm_a"][l])
        c = CV[("hnorm_a", l)]; cv[:, c:c + 8] = _pk(inp["hnorm_a"][l])
        c = CV[("conv_a", l)]
        for j in range(4):
            cv[:, c + j * 8:c + j * 8 + 8] = _pk(inp["conv_a"][l][j])
        c = CV[("norm_b", l)]; cv[:, c:c + 8] = _pk(inp["norm_b"][l])
    c = CV["norm_kv"]; cv[:, c:c + 8] = _pk(inp["norm_kv"])
    for l in range(4):
        c = CV[("ple_norm", l)]; cv[:, c:c + 8] = _pk(inp["ple_norm"][l])
    g["cvec"] = cv
    rw = np.zeros((128, NRW), np.float32)
    for l in range(2):
        c = RW[("b_gate", l)]; rw[:, c:c + 8] = np.asarray(inp["b_gate_a"][l], np.float32)[None, :]
        c = RW[("qnorm", l)]; rw[:, c:c + 128] = np.asarray(inp["qnorm_b"][l], np.float32)[None, :]
    c = RW["knorm"]; rw[:, c:c + 128] = np.asarray(inp["knorm"], np.float32)[None, :]
    g["rows"] = rw
    inv = (np.float32(10000.0) ** (-np.arange(0, 128, 2, dtype=np.float32) / np.float32(128))).astype(np.float32)
    ang = (np.arange(S_LEN, dtype=np.float32)[:, None] * inv[None, :]).astype(np.float32)
    cos = np.cos(ang).astype(np.float32).reshape(NT, 128, 64).transpose(1, 0, 2)
    sin = np.sin(ang).astype(np.float32).reshape(NT, 128, 64).transpose(1, 0, 2)
    g["cos"] = np.ascontiguousarray(cos)
    g["sin"] = np.ascontiguousarray(sin)
    cst = np.zeros((128, 4, 128), np.float32)
    cst[:, 0, :] = np.eye(128, dtype=np.float32)
    cst[:, 1, :] = np.triu(np.ones((128, 128), np.float32))
    cst[:, 2, :] = np.tril(np.ones((128, 128), np.float32))
    cst[:, 3, :] = 1.0
    g["cst"] = cst
    return g


_CACHE = {}


def kernel(**inputs):
    inp = {k: np.asarray(v) for k, v in inputs.items()}
    B = inp["x"].shape[0]
    if "prog" not in _CACHE:
        _CACHE["prog"] = build_program(4, NT)[0]
    nc = _CACHE["prog"]
    g = host_inputs(inp)
    in_maps = []
    for b in range(B):
        m = dict(g)
        m["x"] = np.ascontiguousarray(inp["x"][b], dtype=np.float32)
        m["p"] = np.ascontiguousarray(inp["p"][:, b], dtype=np.float32)
        in_maps.append(m)
    res = run_bass_kernel_spmd(nc, in_maps, core_ids=list(range(B)))
    out = np.stack([np.asarray(r["out"], np.float32) for r in res.results], axis=0)
    return out
```

```python
import contextlib
import math
import numpy as np
import concourse.bass as bass
import concourse.mybir as mybir
from concourse.bass_utils import run_bass_kernel_spmd

F32 = mybir.dt.float32
BF16 = mybir.dt.bfloat16
AF = mybir.ActivationFunctionType
ALU = mybir.AluOpType

S_LEN = 4096
NT = S_LEN // 128
D = 1024
EPS = 1e-6
ENGS = ("pe", "act", "dve", "pool", "sp")


class Op:
    __slots__ = ("eng", "fn", "deps", "need_ms", "ms", "dom", "is_dma", "chan",
                 "ndma", "dval", "clock", "waits", "epoch")


class Sched:
    def __init__(self, nc, same_engine_sync=True):
        self.nc = nc
        self.ops = {e: [] for e in ENGS}
        self.order = []
        self.state = {}
        self.epoch = 0
        self.same_engine_sync = same_engine_sync
        self.chan_count = {}
        self.chans = []
        self.last = {}
        self.pending = {e: [] for e in ENGS}

    def new_epoch(self):
        self.epoch += 1

    def barrier(self):
        deps = list(self.last.values())
        for e in ENGS:
            self.pending[e] = list(deps)

    def _deps_for(self, eng, reads, writes):
        deps = []
        for k in reads:
            st = self.state.get(k)
            if st is not None and st[0] is not None:
                deps.append(st[0])
        for k in writes:
            st = self.state.get(k)
            if st is not None:
                if st[0] is not None and (st[0].is_dma or st[0].eng != eng or eng == "sp"):
                    deps.append(st[0])
                lastr = {}
                for r_ in st[1]:
                    if r_.is_dma:
                        deps.append(r_)
                    elif r_.eng != eng or eng == "sp":
                        lastr[(r_.eng, r_.epoch)] = r_
                deps.extend(lastr.values())
        if self.pending[eng]:
            deps.extend(self.pending[eng])
            self.pending[eng] = []
        return deps

    def _commit(self, op, reads, writes):
        for k in reads:
            st = self.state.setdefault(k, [None, []])
            st[1].append(op)
        for k in writes:
            self.state[k] = [op, []]

    def op(self, eng, fn, reads=(), writes=()):
        o = Op()
        o.eng = eng; o.fn = fn; o.need_ms = False; o.ms = None
        o.is_dma = False; o.epoch = self.epoch
        o.deps = self._deps_for(eng, reads, writes)
        self._commit(o, reads, writes)
        self.ops[eng].append(o)
        self.order.append(o)
        self.last[eng] = o
        return o

    def dma(self, eng, chan, fns, reads=(), writes=()):
        o = Op()
        o.eng = eng; o.fn = fns; o.need_ms = False; o.ms = None
        o.is_dma = True; o.chan = chan; o.ndma = len(fns); o.epoch = self.epoch
        if chan not in self.chan_count:
            self.chan_count[chan] = 0
            self.chans.append(chan)
        self.chan_count[chan] += len(fns)
        o.dval = 16 * self.chan_count[chan]
        o.deps = self._deps_for(eng, reads, writes)
        self._commit(o, reads, writes)
        self.ops[eng].append(o)
        self.order.append(o)
        self.last[("dma", chan)] = o
        return o

    def _skip(self, d, o):
        return (not d.is_dma) and d.eng == o.eng and (not o.is_dma) and \
            (o.eng == "pe" or not self.same_engine_sync)

    def finalize_and_emit(self, final_waits=()):
        nc = self.nc
        for o in self.order:
            for d in o.deps:
                if d.is_dma or self._skip(d, o):
                    continue
                d.need_ms = True
        cnt = {}
        for o in self.order:
            if o.is_dma:
                o.dom = ("dma", o.chan)
                continue
            o.dom = (o.eng, o.epoch)
            if o.need_ms:
                cnt[o.dom] = cnt.get(o.dom, 0) + 1
                o.ms = cnt[o.dom]
        eclock = {e: {} for e in ENGS}
        nwaits = 0
        for o in self.order:
            ck = eclock[o.eng]
            waits = {}
            for d in o.deps:
                if d.is_dma:
                    dom, val = d.dom, d.dval
                else:
                    if self._skip(d, o):
                        continue
                    dom, val = d.dom, d.ms
                if ck.get(dom, 0) >= val:
                    continue
                if waits.get(dom, 0) < val:
                    waits[dom] = val
            for d in o.deps:
                if d.is_dma:
                    dom, val = d.dom, d.dval
                else:
                    dom, val = d.dom, d.ms
                if dom in waits and waits[dom] == val and d.clock is not None:
                    for k, v in d.clock.items():
                        if ck.get(k, 0) < v:
                            ck[k] = v
            for dom, val in waits.items():
                if ck.get(dom, 0) < val:
                    ck[dom] = val
            o.waits = list(waits.items())
            nwaits += len(o.waits)
            if o.is_dma:
                c = dict(ck); c[o.dom] = o.dval
                o.clock = c
            elif o.ms is not None:
                c = dict(ck); c[o.dom] = o.ms
                if o.eng == "pe" or not self.same_engine_sync:
                    ck[o.dom] = o.ms
                o.clock = c
            else:
                o.clock = None
        self.nwaits = nwaits
        stack = contextlib.ExitStack()
        sems = {}
        doms = list(cnt.keys()) + [("dma", c) for c in self.chans]
        for i, dom in enumerate(doms):
            sems[dom] = stack.enter_context(nc.semaphore("s%d" % i))
        fw = [(sems[d.dom], d.dval) for d in final_waits]
        ops = self.ops
        with stack:
            with nc.Block() as block:
                def emit(engname, engobj, last=False):
                    for o in ops[engname]:
                        for dom, val in o.waits:
                            engobj.wait_ge(sems[dom], val)
                        if o.is_dma:
                            for f in o.fn:
                                f(engobj).then_inc(sems[o.dom], 16)
                        else:
                            ins = o.fn(engobj)
                            if o.ms is not None:
                                ins.then_inc(sems[o.dom], 1)
                    if last:
                        for s, v in fw:
                            engobj.wait_ge(s, v)

                @block.tensor
                def _(e):
                    emit("pe", e)

                @block.scalar
                def _(e):
                    emit("act", e)

                @block.vector
                def _(e):
                    emit("dve", e)

                @block.gpsimd
                def _(e):
                    emit("pool", e)

                @block.sync
                def _(e):
                    emit("sp", e, last=True)


def _pk(v):
    return np.ascontiguousarray(np.asarray(v, np.float32).reshape(-1, 128).T)


CV = {}
_c = 0
for _l in range(2):
    CV[("norm_a", _l)] = _c; _c += 8
    CV[("hnorm_a", _l)] = _c; _c += 8
    CV[("conv_a", _l)] = _c; _c += 32
CV["norm_kv"] = _c; _c += 8
for _l in range(2):
    CV[("norm_b", _l)] = _c; _c += 8
for _l in range(4):
    CV[("ple_norm", _l)] = _c; _c += 8
NCV = _c
RW = {}
_c = 0
for _l in range(2):
    RW[("b_gate", _l)] = _c; _c += 8
RW["knorm"] = _c; _c += 128
for _l in range(2):
    RW[("qnorm", _l)] = _c; _c += 128
NRW = _c
LNSK = math.log(128.0 ** -0.5)


def build_program(n_layers=4, ntiles=NT):
    nc = bass.Bass("TRN2", target_bir_lowering=False)
    S_ = ntiles * 128

    def din(name, shape):
        return nc.dram_tensor(name, shape, F32, kind="ExternalInput").ap()

    x_d = din("x", [S_LEN, D])
    p_d = din("p", [4, S_LEN, 256])
    w_in_a_d = din("w_in_a", [2, 128, 8, 4104])
    w_out_a_d = din("w_out_a", [2, 128, 8, 1024])
    w_kv_d = din("w_kv", [128, 8, 1024])
    w_in_b_d = din("w_in_b", [2, 128, 8, 2048])
    w_out_b_d = din("w_out_b", [2, 128, 4, 1024])
    w_pg_d = din("w_ple_gate", [4, 128, 8, 1024])
    w_pp_d = din("w_ple", [4, 128, 2, 1024])
    cvec_d = din("cvec", [128, NCV])
    rows_d = din("rows", [128, NRW])
    cos_d = din("cos", [128, NT, 64])
    sin_d = din("sin", [128, NT, 64])
    cst_d = din("cst", [128, 4, 128])
    out_d = nc.dram_tensor("out", [S_LEN, D], F32, kind="ExternalOutput").ap()
    hA_d = nc.dram_tensor("hA", [S_LEN, D], F32).ap()
    hB_d = nc.dram_tensor("hB", [S_LEN, D], F32).ap()
    qT_d = nc.dram_tensor("qT", [4, 128, 3, S_LEN], BF16).ap()

    S = Sched(nc)
    OP = S.op
    gs = contextlib.ExitStack()

    uid = [0]

    def sb(es, name, shape, dt):
        uid[0] += 1
        return es.enter_context(nc.sbuf_tensor("%s_u%d" % (name, uid[0]), shape, dt))

    cnt = {"b": 0, "f": 0, "n": 0, "stg": 0, "cv": 0}

    with gs:
        cstf = sb(gs, "cstf", [128, 4, 128], F32)
        identb = sb(gs, "identb", [128, 128], BF16)
        triub = sb(gs, "triub", [128, 128], BF16)
        mask2 = sb(gs, "mask2", [128, 4, 128], BF16)
        onesb = sb(gs, "onesb", [128, 128], BF16)
        cvec = sb(gs, "cvec", [128, NCV], F32)
        rows = sb(gs, "rows", [128, NRW], F32)
        PSB = [gs.enter_context(nc.psum_tensor("psb%d" % i, [128, 1024], BF16)) for i in range(2)]
        PSF = [gs.enter_context(nc.psum_tensor("psf%d" % i, [128, 512], F32)) for i in range(6)]
        triuf = cstf[:, 1, :]
        onesf = cstf[:, 3, :]

        def nextb():
            i = cnt["b"] % 2; cnt["b"] += 1
            return PSB[i], ("pb", i)

        def nextf():
            i = cnt["f"] % 6; cnt["f"] += 1
            return PSF[i], ("pf", i)

        S.dma("sp", "cst", [lambda e: e.dma_start(out=cstf[:], in_=cst_d),
                            lambda e: e.dma_start(out=cvec[:], in_=cvec_d),
                            lambda e: e.dma_start(out=rows[:], in_=rows_d)],
              writes=["cstf", "cvec", "rows"])
        OP("dve", lambda e: e.tensor_copy(out=identb[:], in_=cstf[:, 0, :]), ["cstf"], ["identb"])
        OP("dve", lambda e: e.tensor_copy(out=triub[:], in_=cstf[:, 1, :]), ["cstf"], ["triub"])
        OP("dve", lambda e: e.tensor_copy(out=onesb[:], in_=cstf[:, 3, :]), ["cstf"], ["onesb"])
        for i in range(4):
            OP("dve", lambda e, i=i: e.tensor_copy(out=mask2[:, i, :], in_=cstf[:, 1 + (i % 2), :]),
               ["cstf"], ["mask2"])

        def mm(out, lhsT, rhs, start, stop, r, w):
            OP("pe", lambda e: e.matmul(out=out, lhsT=lhsT, rhs=rhs, start=start, stop=stop), r, w)

        def sigmoid_inplace(dst, src, rkeys, wkey):
            OP("act", lambda e: e.activation(out=dst, in_=src, func=AF.Exp, scale=-1.0), rkeys, [wkey])
            OP("act", lambda e: e.activation(out=dst, in_=dst, func=AF.Ln, bias=1.0), [wkey], [wkey])
            OP("act", lambda e: e.activation(out=dst, in_=dst, func=AF.Exp, scale=-1.0), [wkey], [wkey])

        def load_weights(specs, nstg=4):
            with contextlib.ExitStack() as ws:
                stg = [sb(ws, "stgL%d" % i, [128, 4104], F32) for i in range(nstg)]
                for dst, src, K, N, gcol, wkey in specs:
                    kk = max(1, 4104 // N)
                    for k0 in range(0, K, kk):
                        k1 = min(K, k0 + kk)
                        si = cnt["stg"] % nstg; cnt["stg"] += 1
                        sv = stg[si][:, 0:(k1 - k0) * N].rearrange("p (k n) -> p k n", k=k1 - k0)
                        S.dma("sp", ("stg", si),
                              [lambda e, sv=sv, src=src, k0=k0, k1=k1: e.dma_start(out=sv, in_=src[:, k0:k1, :])],
                              writes=[("stg", si)])
                        for k in range(k0, k1):
                            eng = ("act", "dve")[cnt["cv"] % 2]; cnt["cv"] += 1
                            o_ap = dst[:, k, :]
                            i_ap = sv[:, k - k0, :]
                            if gcol is None:
                                if eng == "act":
                                    fn = lambda e, o_ap=o_ap, i_ap=i_ap: e.activation(out=o_ap, in_=i_ap, func=AF.Copy)
                                else:
                                    fn = lambda e, o_ap=o_ap, i_ap=i_ap: e.tensor_copy(out=o_ap, in_=i_ap)
                            else:
                                g_ap = cvec[:, gcol + k:gcol + k + 1]
                                if eng == "act":
                                    fn = lambda e, o_ap=o_ap, i_ap=i_ap, g_ap=g_ap: e.activation(
                                        out=o_ap, in_=i_ap, func=AF.Copy, scale=g_ap)
                                else:
                                    fn = lambda e, o_ap=o_ap, i_ap=i_ap, g_ap=g_ap: e.tensor_scalar(
                                        out=o_ap, in0=i_ap, scalar1=g_ap, scalar2=None, op0=ALU.mult)
                            OP(eng, fn, [("stg", si), "cvec"], [(wkey, eng)])
                S.barrier()

        def wk(name):
            return [(name, "act"), (name, "dve")]

        def norm_T(bufs, src, skey, slot=None, dim=D):
            if slot is None:
                i = cnt["n"] % 2; cnt["n"] += 1
            else:
                i = slot
            junk, ss, rstd, xs, xsT = bufs["junk"], bufs["ss"], bufs["rstd"], bufs["xs"], bufs["xsT"]
            OP("act", lambda e: e.activation(out=junk[:], in_=src, func=AF.Square, accum_out=ss[:, i:i + 1]),
               [skey], ["junk", ("ss", i)])
            OP("act", lambda e: e.activation(out=rstd[:, i:i + 1], in_=ss[:, i:i + 1], func=AF.Ln,
                                             scale=1.0 / dim, bias=EPS), [("ss", i)], [("rstd", i)])
            OP("act", lambda e: e.activation(out=rstd[:, i:i + 1], in_=rstd[:, i:i + 1], func=AF.Exp, scale=-0.5),
               [("rstd", i)], [("rstd", i)])
            OP("dve", lambda e: e.tensor_scalar(out=xs[i][:], in0=src, scalar1=rstd[:, i:i + 1], scalar2=None,
                                                op0=ALU.mult), [skey, ("rstd", i)], [("xs", i)])
            pb, pbk = nextb()
            for k in range(8):
                OP("pe", lambda e, k=k: e.transpose(out=pb[:, k * 128:(k + 1) * 128],
                                                    in_=xs[i][:, k * 128:(k + 1) * 128], identity=identb[:]),
                   [("xs", i), "identb"], [pbk])
            OP("act", lambda e: e.activation(out=xsT[i][:].rearrange("p k t -> p (k t)"), in_=pb[:], func=AF.Copy),
               [pbk], [("xsT", i)])
            return xsT[i], ("xsT", i)

        def alloc_ple_w(es):
            b = {}
            b["Wg"] = sb(es, "Wg", [128, 8, 1024], BF16)
            b["Wp"] = sb(es, "Wp", [128, 2, 1024], BF16)
            return b

        def alloc_common(es, b):
            b["ht"] = [sb(es, "ht%d" % i, [128, D], F32) for i in range(3)]
            b["junk"] = sb(es, "junk", [128, D], BF16)
            b["ss"] = sb(es, "ss", [128, 4], F32)
            b["rstd"] = sb(es, "rstd", [128, 4], F32)
            b["xs"] = [sb(es, "xs%d" % i, [128, D], BF16) for i in range(3)]
            b["xsT"] = [sb(es, "xsT%d" % i, [128, 8, 128], BF16) for i in range(3)]
            b["pt"] = [sb(es, "pt%d" % i, [128, 256], F32) for i in range(2)]
            b["pb16"] = sb(es, "pb16", [128, 256], BF16)
            b["pT"] = sb(es, "pT", [128, 2, 128], BF16)
            b["TP"] = sb(es, "TP", [128, D], F32)
            return b

        def load_h(bufs, hsrc, t):
            hs = t % 3
            S.dma("sp", ("ht", hs),
                  [lambda e: e.dma_start(out=bufs["ht"][hs][:], in_=hsrc[t * 128:(t + 1) * 128, :])],
                  writes=[("ht", hs)])

        def load_p(bufs, l, t):
            s2 = t % 2
            S.dma("sp", ("pt", s2),
                  [lambda e: e.dma_start(out=bufs["pt"][s2][:], in_=p_d[l, t * 128:(t + 1) * 128, :])],
                  writes=[("pt", s2)])

        def load_tile(bufs, l, hsrc, t):
            hs = t % 3
            S.dma("sp", ("ht", hs),
                  [lambda e: e.dma_start(out=bufs["ht"][hs][:], in_=hsrc[t * 128:(t + 1) * 128, :])],
                  writes=[("ht", hs)])
            s2 = t % 2
            S.dma("sp", ("pt", s2),
                  [lambda e: e.dma_start(out=bufs["pt"][s2][:], in_=p_d[l, t * 128:(t + 1) * 128, :])],
                  writes=[("pt", s2)])

        def ple_and_store(bufs, l, t, hdst):
            hs = t % 3
            X = bufs["ht"][hs]
            hk = ("ht", hs)
            s2 = t % 2
            xT2, xT2k = norm_T(bufs, X[:], hk, slot=2)
            pb16, pT, TP = bufs["pb16"], bufs["pT"], bufs["TP"]
            OP("dve", lambda e: e.tensor_copy(out=pb16[:], in_=bufs["pt"][s2][:]), [("pt", s2)], ["pb16"])
            pb, pbk = nextb()
            for k in range(2):
                OP("pe", lambda e, k=k: e.transpose(out=pb[:, k * 128:(k + 1) * 128],
                                                    in_=pb16[:, k * 128:(k + 1) * 128], identity=identb[:]),
                   ["pb16", "identb"], [pbk])
            OP("dve", lambda e: e.tensor_copy(out=pT[:].rearrange("p k t -> p (k t)"), in_=pb[:, 0:256]),
               [pbk], ["pT"])
            Wg, Wp = bufs["Wg"], bufs["Wp"]
            for nb in range(2):
                sl = slice(nb * 512, (nb + 1) * 512)
                pg, pgk = nextf()
                for k in range(8):
                    mm(pg[:], xT2[:, k, :], Wg[:, k, sl], k == 0, k == 7, [xT2k] + wk("Wg"), [pgk])
                pp, ppk = nextf()
                for k in range(2):
                    mm(pp[:], pT[:, k, :], Wp[:, k, sl], k == 0, k == 1, ["pT"] + wk("Wp"), [ppk])
                tk = ("TP", nb)
                sigmoid_inplace(TP[:, sl], pg[:], [pgk], tk)
                OP("dve", lambda e, sl=sl, pp=pp: e.tensor_tensor(out=TP[:, sl], in0=pp[:], in1=TP[:, sl],
                                                                 op=ALU.mult), [ppk, tk], [tk])
                OP("dve", lambda e, sl=sl: e.tensor_tensor(out=X[:, sl], in0=X[:, sl], in1=TP[:, sl], op=ALU.add),
                   [hk, tk], [hk])
            return S.dma("pool", ("st", hs),
                         [lambda e: e.dma_start(out=hdst[t * 128:(t + 1) * 128, :], in_=X[:])], reads=[hk])

        def layer_A(l, hsrc, hdst):
            S.new_epoch()
            S.barrier()
            es = contextlib.ExitStack()
            with es:
                bufs = alloc_ple_w(es)
                Win = sb(es, "Win", [128, 8, 4104], BF16)
                Wout = sb(es, "Wout", [128, 8, 1024], BF16)
                load_weights([(Win, w_in_a_d[l], 8, 4104, CV[("norm_a", l)], "Win"),
                              (Wout, w_out_a_d[l], 8, 1024, CV[("hnorm_a", l)], "Wout"),
                              (bufs["Wg"], w_pg_d[l], 8, 1024, CV[("ple_norm", l)], "Wg"),
                              (bufs["Wp"], w_pp_d[l], 2, 1024, None, "Wp")])
                alloc_common(es, bufs)
                xq = sb(es, "xq", [128, 8, 131], F32)
                T0 = sb(es, "T0", [128, D], F32)
                T1 = sb(es, "T1", [128, D], F32)
                T2 = [sb(es, "T2_%d" % i, [128, D], BF16) for i in range(2)]
                T3 = [sb(es, "T3_%d" % i, [128, D], BF16) for i in range(2)]
                T3sb = [sb(es, "T3s%d" % i, [128, 512], F32) for i in range(2)]
                qkT = [sb(es, "qkT%d" % i, [128, 8, 128], BF16) for i in range(2)]
                Ktok = [sb(es, "Ktok%d" % i, [128, 512], BF16) for i in range(2)]
                uV = [sb(es, "uV%d" % i, [128, 4, 258], BF16) for i in range(2)]
                PT = sb(es, "PT", [128, 4, 128], BF16)
                ho = sb(es, "ho", [128, D], F32)
                yb = sb(es, "yb", [128, D], BF16)
                yT = sb(es, "yT", [128, 8, 128], BF16)
                Dst = sb(es, "Dst", [128, 4, 257], F32)
                Cb = sb(es, "Cb", [128, 4, 258], BF16)
                sm = sb(es, "sm", [128, 96], F32)
                g8 = sm[:, 0:8]; l4 = sm[:, 8:12]; iB = sm[:, 16:20]
                u4 = sm[:, 20:24]; t1 = sm[:, 24:28]; r4 = sm[:, 28:32]; ssh = sm[:, 32:36]
                rsh = sm[:, 36:40]
                E3c = [40, 44, 48]
                eBc = [52, 56]
                T0v = T0[:].rearrange("p (c t) -> p c t", c=8)
                T1v = T1[:].rearrange("p (c t) -> p c t", c=8)
                OP("pool", lambda e: e.memset(xq[:, :, 0:3], 0.0), [], [("xq", 0), ("xq", 1)])
                cb = CV[("conv_a", l)]
                bg = RW[("b_gate", l)]
                ctx = {}

                def s0(t):
                    hs = t % 3
                    X = bufs["ht"][hs]
                    ctx[t] = norm_T(bufs, X[:], ("ht", hs), slot=t % 2)

                def s1(t):
                    i2 = t % 2
                    xT, xTk = ctx.pop(t)
                    eBo = eBc[i2]
                    Eo = E3c[t % 3]
                    eB = sm[:, eBo:eBo + 4]
                    Ecur = sm[:, Eo:Eo + 4]
                    pfg, pfgk = nextf()
                    for k in range(8):
                        mm(pfg[:, 0:8], xT[:, k, :], Win[:, k, 4096:4104], k == 0, k == 7, [xTk] + wk("Win"), [pfgk])
                    OP("dve", lambda e: e.tensor_tensor(out=g8, in0=pfg[:, 0:8], in1=rows[:, bg:bg + 8],
                                                        op=ALU.add), [pfgk, "rows"], ["g8"])
                    OP("act", lambda e: e.activation(out=l4, in_=sm[:, 4:8], func=AF.Exp, scale=-1.0), ["g8"], ["l4"])
                    OP("act", lambda e: e.activation(out=l4, in_=l4, func=AF.Ln, bias=1.0), ["l4"], ["l4"])
                    mm(pfg[:, 8:12], triuf, l4, True, True, ["cstf", "l4"], [pfgk])
                    mm(pfg[:, 12:16], onesf, l4, True, True, ["cstf", "l4"], [pfgk])
                    OP("act", lambda e: e.activation(out=eB, in_=pfg[:, 8:12], func=AF.Exp, scale=-1.0),
                       [pfgk], [("eB", i2)])
                    OP("dve", lambda e: e.tensor_tensor(out=iB, in0=sm[:, 0:4], in1=pfg[:, 8:12], op=ALU.add),
                       [pfgk, "g8"], ["iB"])
                    OP("act", lambda e: e.activation(out=u4, in_=iB, func=AF.Exp, bias=LNSK), ["iB"], ["u4"])
                    OP("act", lambda e: e.activation(out=Ecur, in_=pfg[:, 12:16], func=AF.Exp, scale=-1.0),
                       [pfgk], [("E", t % 3)])
                    if t > 0:
                        OP("dve", lambda e: e.tensor_copy(out=xq[:, :, 0:3], in_=xq[:, :, 128:131]),
                           [("xq", 0), ("xq", 1)], [("xq", 0), ("xq", 1)])
                    for half in range(2):
                        pf, pfk = nextf()
                        for c4 in range(4):
                            c = half * 4 + c4
                            for k in range(8):
                                mm(pf[:, c4 * 128:(c4 + 1) * 128], Win[:, k, c * 128:(c + 1) * 128], xT[:, k, :],
                                   k == 0, k == 7, [xTk] + wk("Win"), [pfk])
                        OP("act", lambda e, pf=pf, half=half: e.activation(
                            out=xq[:, half * 4:half * 4 + 4, 3:131],
                            in_=pf[:].rearrange("p (c t) -> p c t", c=4), func=AF.Copy), [pfk], [("xq", half)])

                    def wc(j):
                        return cvec[:, cb + j * 8:cb + j * 8 + 8].unsqueeze(2).to_broadcast([128, 8, 128])
                    xqk = [("xq", 0), ("xq", 1)]
                    OP("dve", lambda e: e.tensor_tensor(out=T0v, in0=xq[:, :, 3:131], in1=wc(3), op=ALU.mult),
                       xqk + ["cvec"], ["T0"])
                    for j in (2, 1, 0):
                        OP("dve", lambda e, j=j: e.tensor_tensor(out=T1v, in0=xq[:, :, j:j + 128], in1=wc(j),
                                                                op=ALU.mult), xqk + ["cvec"], ["T1"])
                        OP("dve", lambda e: e.tensor_tensor(out=T0[:], in0=T0[:], in1=T1[:], op=ALU.add),
                           ["T0", "T1"], ["T0"])
                    sigmoid_inplace(T1[:], T0[:], ["T0"], "T1")
                    qk = qkT[i2]
                    qkk = ("qkT", i2)
                    OP("dve", lambda e: e.tensor_tensor(out=qk[:].rearrange("p c t -> p (c t)"), in0=T0[:],
                                                        in1=T1[:], op=ALU.mult), ["T0", "T1"], [qkk])
                    pb, pbk = nextb()
                    for h in range(4):
                        OP("pe", lambda e, h=h: e.transpose(out=pb[:, h * 128:(h + 1) * 128],
                                                            in_=qk[:, 4 + h, :], identity=identb[:]),
                           [qkk, "identb"], [pbk])
                    Kt = Ktok[i2]
                    Ktk = ("Ktok", i2)
                    OP("dve", lambda e: e.tensor_copy(out=Kt[:], in_=pb[:, 0:512]), [pbk], [Ktk])
                    uVc = uV[i2]
                    uVk = ("uV", i2)
                    T2c = T2[i2]
                    T3c = T3[i2]
                    for nb in range(6):
                        pf, pfk = nextf()
                        for k in range(8):
                            mm(pf[:], xT[:, k, :], Win[:, k, 1024 + nb * 512:1024 + (nb + 1) * 512],
                               k == 0, k == 7, [xTk] + wk("Win"), [pfk])
                        if nb < 2:
                            for hh in range(2):
                                h = 2 * nb + hh
                                OP("act", lambda e, h=h, hh=hh, pf=pf: e.activation(
                                    out=uVc[:, h, 0:256], in_=pf[:, hh * 256:(hh + 1) * 256], func=AF.Copy,
                                    scale=sm[:, 20 + h:21 + h]), [pfk, "u4"], [uVk])
                        elif nb < 4:
                            sl = slice((nb - 2) * 512, (nb - 1) * 512)
                            T3s = T3sb[nb % 2]; tsk = ("T3s", nb % 2)
                            sigmoid_inplace(T3s[:], pf[:], [pfk], tsk)
                            OP("dve", lambda e, sl=sl, T3s=T3s: e.tensor_copy(out=T2c[:, sl], in_=T3s[:]),
                               [tsk], [("T2", i2, nb - 2)])
                        else:
                            sl = slice((nb - 4) * 512, (nb - 3) * 512)
                            T3s = T3sb[nb % 2]; tsk = ("T3s", nb % 2)
                            sigmoid_inplace(T3s[:], pf[:], [pfk], tsk)
                            OP("dve", lambda e, sl=sl, pf=pf, T3s=T3s: e.tensor_tensor(out=T3c[:, sl], in0=pf[:],
                                                                                      in1=T3s[:], op=ALU.mult),
                               [pfk, tsk], [("T3", i2, nb - 4)])
                    OP("dve", lambda e: e.tensor_copy(out=uVc[:, :, 256], in_=u4), ["u4"], [uVk])

                def s2(t):
                    i2 = t % 2
                    eBo = eBc[i2]
                    eB = sm[:, eBo:eBo + 4]
                    Eo = E3c[t % 3]
                    Ecur = sm[:, Eo:Eo + 4]
                    Epo = E3c[(t - 1) % 3]
                    qk = qkT[i2]; qkk = ("qkT", i2)
                    Kt = Ktok[i2]; Ktk = ("Ktok", i2)
                    uVc = uV[i2]; uVk = ("uV", i2)
                    T2c = T2[i2]; T3c = T3[i2]
                    pS, pSk = nextf()
                    for h in range(4):
                        mm(pS[:, h * 128:(h + 1) * 128], qk[:, 4 + h, :], qk[:, h, :], True, True, [qkk], [pSk])
                    OP("dve", lambda e: e.tensor_tensor(
                        out=PT[:], in0=pS[:].rearrange("p (h t) -> p h t", h=4),
                        in1=triub[:].unsqueeze(1).to_broadcast([128, 4, 128]), op=ALU.mult), [pSk, "triub"], ["PT"])
                    pas = []
                    for h in range(4):
                        pa, pak = nextf()
                        pas.append((pa, pak))
                        mm(pa[:, 0:257], PT[:, h, :], uVc[:, h, 0:257], True, t == 0, ["PT", uVk], [pak])
                        if t > 0:
                            mm(pa[:, 0:257], qk[:, h, :], Cb[:, h, 0:257], False, True, [qkk, "Cb"], [pak])
                        OP("dve", lambda e, h=h, pa=pa: e.tensor_tensor(out=sm[:, 24 + h:25 + h],
                                                                       in0=sm[:, eBo + h:eBo + h + 1],
                                                                       in1=pa[:, 256:257], op=ALU.mult),
                           [pak, ("eB", i2)], [("t1", h)])
                    pus = []
                    for h in range(2):
                        pu, puk = nextf()
                        pus.append((pu, puk))
                        mm(pu[:, 0:257], Kt[:, h * 128:(h + 1) * 128], uVc[:, h, 0:257], True, True, [Ktk, uVk], [puk])
                    t1k = [("t1", h) for h in range(4)]
                    OP("act", lambda e: e.activation(out=t1, in_=t1, func=AF.Abs), t1k, t1k)
                    OP("dve", lambda e: e.tensor_scalar(out=t1, in0=t1, scalar1=1.0, scalar2=None, op0=ALU.max),
                       t1k, t1k)
                    OP("dve", lambda e: e.reciprocal(out=t1, in_=t1), t1k, t1k)
                    OP("dve", lambda e: e.tensor_tensor(out=r4, in0=eB, in1=t1, op=ALU.mult),
                       t1k + [("eB", i2)], ["r4"])
                    for h in range(4):
                        pa, pak = pas[h]
                        hsl = slice(h * 256, (h + 1) * 256)
                        OP("dve", lambda e, h=h, pa=pa, hsl=hsl: e.scalar_tensor_tensor(
                            out=ho[:, hsl], in0=pa[:, 0:256], scalar=sm[:, 28 + h:29 + h], in1=T2c[:, hsl],
                            op0=ALU.mult, op1=ALU.mult), [pak, "r4", ("T2", i2, h // 2)], [("ho", h)])
                        OP("act", lambda e, h=h, hsl=hsl: e.activation(out=bufs["junk"][:, 0:256], in_=ho[:, hsl],
                                                                      func=AF.Square, accum_out=sm[:, 32 + h:33 + h]),
                           [("ho", h)], ["junk", ("ssh", h)])
                    for h in range(2, 4):
                        pu, puk = nextf()
                        pus.append((pu, puk))
                        mm(pu[:, 0:257], Kt[:, h * 128:(h + 1) * 128], uVc[:, h, 0:257], True, True, [Ktk, uVk], [puk])
                    for h in range(4):
                        pu, puk = pus[h]
                        if t == 0:
                            OP("dve", lambda e, h=h, pu=pu: e.tensor_copy(out=Dst[:, h, :], in_=pu[:, 0:257]),
                               [puk], [("D", h)])
                        else:
                            OP("dve", lambda e, h=h, pu=pu: e.scalar_tensor_tensor(
                                out=Dst[:, h, :], in0=Dst[:, h, :], scalar=sm[:, Epo + h:Epo + h + 1], in1=pu[:, 0:257],
                                op0=ALU.mult, op1=ALU.add), [puk, ("D", h), ("E", (t - 1) % 3)], [("D", h)])
                        OP("act", lambda e, h=h: e.activation(out=Cb[:, h, 0:257], in_=Dst[:, h, :],
                                                              func=AF.Copy, scale=sm[:, Eo + h:Eo + h + 1]),
                           [("D", h), ("E", t % 3)], ["Cb"])
                    sshk = [("ssh", h) for h in range(4)]
                    OP("act", lambda e: e.activation(out=rsh, in_=ssh, func=AF.Ln, scale=1.0 / 256, bias=EPS),
                       sshk, ["rsh"])
                    OP("act", lambda e: e.activation(out=rsh, in_=rsh, func=AF.Exp, scale=-0.5), ["rsh"], ["rsh"])
                    for h in range(4):
                        hsl = slice(h * 256, (h + 1) * 256)
                        OP("dve", lambda e, h=h, hsl=hsl: e.scalar_tensor_tensor(
                            out=yb[:, hsl], in0=ho[:, hsl], scalar=sm[:, 36 + h:37 + h], in1=T3c[:, hsl],
                            op0=ALU.mult, op1=ALU.mult), [("ho", h), "rsh", ("T3", i2, h // 2)], [("yb", h)])

                def s3(t):
                    hs = t % 3
                    X = bufs["ht"][hs]
                    hk = ("ht", hs)
                    pb, pbk = nextb()
                    for k in range(8):
                        OP("pe", lambda e, k=k: e.transpose(out=pb[:, k * 128:(k + 1) * 128],
                                                            in_=yb[:, k * 128:(k + 1) * 128], identity=identb[:]),
                           [("yb", k // 2), "identb"], [pbk])
                    OP("act", lambda e: e.activation(out=yT[:].rearrange("p k t -> p (k t)"), in_=pb[:],
                                                     func=AF.Copy), [pbk], ["yT"])
                    for nb in range(2):
                        sl = slice(nb * 512, (nb + 1) * 512)
                        pf, pfk = nextf()
                        for k in range(8):
                            mm(pf[:], yT[:, k, :], Wout[:, k, sl], k == 0, k == 7, ["yT"] + wk("Wout"), [pfk])
                        OP("dve", lambda e, sl=sl, pf=pf: e.tensor_tensor(out=X[:, sl], in0=X[:, sl], in1=pf[:],
                                                                         op=ALU.add), [pfk, hk], [hk])
                    ple_and_store(bufs, l, t, hdst)

                load_h(bufs, hsrc, 0)
                if ntiles > 1:
                    load_h(bufs, hsrc, 1)
                load_p(bufs, l, 0)
                s0(0)
                s1(0)
                if ntiles > 1:
                    s0(1)
                for t in range(ntiles):
                    if t + 2 < ntiles:
                        load_h(bufs, hsrc, t + 2)
                    if t + 1 < ntiles:
                        load_p(bufs, l, t + 1)
                        s1(t + 1)
                    s2(t)
                    if t + 2 < ntiles:
                        s0(t + 2)
                    s3(t)
                S.barrier()

        SCALE = 128.0 ** -0.5
        AX = mybir.AxisListType.X

        def headnorm_rope(tb, pf, pfk, nrm_col, t, outv, outk):
            tid = tb.get("id", 0)
            sq, qf, ssq, rsq, ta, tbb, cosT, sinT = (tb["sq"], tb["qf"], tb["ssq"], tb["rsq"], tb["ta"], tb["tb"],
                                                     tb["cos"], tb["sin"])
            ksq, kqf, kss, krs, kta, ktb = [(n, tid) for n in ("sq", "qf", "ssq", "rsq", "ta", "tb")]
            pv = pf[:].rearrange("p (h d) -> p h d", h=4)
            qv = qf[:].rearrange("p (h d) -> p h d", h=4)
            OP("act", lambda e: e.activation(out=sq[:], in_=pf[:], func=AF.Square), [pfk], [ksq])
            OP("dve", lambda e: e.tensor_reduce(out=ssq[:], in_=sq[:].rearrange("p (h d) -> p h d", h=4), axis=AX,
                                                op=ALU.add), [ksq], [kss])
            OP("act", lambda e: e.activation(out=rsq[:], in_=ssq[:], func=AF.Ln, scale=1.0 / 128, bias=EPS),
               [kss], [krs])
            OP("act", lambda e: e.activation(out=rsq[:], in_=rsq[:], func=AF.Exp, scale=-0.5), [krs], [krs])
            OP("dve", lambda e: e.tensor_tensor(out=qv, in0=pv, in1=rsq[:].unsqueeze(2).to_broadcast([128, 4, 128]),
                                                op=ALU.mult), [pfk, krs], [kqf])
            OP("dve", lambda e: e.tensor_tensor(
                out=qv, in0=qv, in1=rows[:, nrm_col:nrm_col + 128].unsqueeze(1).to_broadcast([128, 4, 128]),
                op=ALU.mult), [kqf, "rows"], [kqf])
            cb_ = cosT[:, t, :].unsqueeze(1).to_broadcast([128, 4, 64])
            sb_ = sinT[:, t, :].unsqueeze(1).to_broadcast([128, 4, 64])
            x1 = qv[:, :, 0:64]
            x2 = qv[:, :, 64:128]
            OP("dve", lambda e: e.tensor_tensor(out=ta[:], in0=x1, in1=cb_, op=ALU.mult), [kqf, "cs"], [kta])
            OP("dve", lambda e: e.tensor_tensor(out=tbb[:], in0=x2, in1=sb_, op=ALU.mult), [kqf, "cs"], [ktb])
            OP("dve", lambda e: e.tensor_tensor(out=outv[:, :, 0:64], in0=ta[:], in1=tbb[:], op=ALU.subtract),
               [kta, ktb], [outk])
            OP("dve", lambda e: e.tensor_tensor(out=ta[:], in0=x2, in1=cb_, op=ALU.mult), [kqf, "cs"], [kta])
            OP("dve", lambda e: e.tensor_tensor(out=tbb[:], in0=x1, in1=sb_, op=ALU.mult), [kqf, "cs"], [ktb])
            OP("dve", lambda e: e.tensor_tensor(out=outv[:, :, 64:128], in0=ta[:], in1=tbb[:], op=ALU.add),
               [kta, ktb], [outk])

        def phase_B1(l, j, hsrc, KT, VT):
            S.new_epoch()
            S.barrier()
            es = contextlib.ExitStack()
            with es:
                bufs = {}
                Wq = sb(es, "Wq", [128, 8, 1536], BF16)
                specs = [(Wq, w_in_b_d[j][:, :, 0:1536], 8, 1536, CV[("norm_b", j)], "Wq")]
                if j == 0:
                    Wkv = sb(es, "Wkv", [128, 8, 1024], BF16)
                    specs.append((Wkv, w_kv_d, 8, 1024, CV["norm_kv"], "Wkv"))
                load_weights(specs)
                bufs["ht"] = [sb(es, "ht%d" % i, [128, D], F32) for i in range(3)]
                bufs["junk"] = sb(es, "junk", [128, D], BF16)
                bufs["ss"] = sb(es, "ss", [128, 2], F32)
                bufs["rstd"] = sb(es, "rstd", [128, 2], F32)
                bufs["xs"] = [sb(es, "xs%d" % i, [128, D], BF16) for i in range(2)]
                bufs["xsT"] = [sb(es, "xsT%d" % i, [128, 8, 128], BF16) for i in range(2)]
                cosT = sb(es, "cosT", [128, NT, 64], F32)
                sinT = sb(es, "sinT", [128, NT, 64], F32)
                tbs = []
                for i in range(2):
                    tb = {"id": i, "cos": cosT, "sin": sinT}
                    tb["sq"] = sb(es, "sq", [128, 512], F32)
                    tb["qf"] = sb(es, "qf", [128, 512], F32)
                    tb["ssq"] = sb(es, "ssq", [128, 4], F32)
                    tb["rsq"] = sb(es, "rsq", [128, 4], F32)
                    tb["ta"] = sb(es, "ta", [128, 4, 64], F32)
                    tb["tb"] = sb(es, "tb", [128, 4, 64], F32)
                    tbs.append(tb)
                qr = [sb(es, "qr%d" % i, [128, 12, 128], BF16) for i in range(2)]
                kr = [sb(es, "kr%d" % i, [128, 4, 128], BF16) for i in range(2)]
                qTs = [sb(es, "qTs%d" % i, [128, 12, 128], BF16) for i in range(2)]
                S.dma("sp", "cs", [lambda e: e.dma_start(out=cosT[:], in_=cos_d),
                                   lambda e: e.dma_start(out=sinT[:], in_=sin_d)], writes=["cs"])

                def ld(t):
                    hs = t % 3
                    S.dma("sp", ("ht", hs),
                          [lambda e: e.dma_start(out=bufs["ht"][hs][:], in_=hsrc[t * 128:(t + 1) * 128, :])],
                          writes=[("ht", hs)])
                qn = RW[("qnorm", j)]
                ctx = {}
                chain = [0]

                def Sa(t):
                    hs = t % 3
                    ctx[("x", t)] = norm_T(bufs, bufs["ht"][hs][:], ("ht", hs), slot=t % 2)

                def Sb(t):
                    xT, xTk = ctx.pop(("x", t))
                    banks = {}
                    if j == 0:
                        pf, pfk = nextf()
                        for c in range(4):
                            for k in range(8):
                                mm(pf[:, c * 128:(c + 1) * 128], Wkv[:, k, 512 + c * 128:512 + (c + 1) * 128],
                                   xT[:, k, :], k == 0, k == 7, [xTk] + wk("Wkv"), [pfk])
                        banks["v"] = (pf, pfk)
                        pf, pfk = nextf()
                        for k in range(8):
                            mm(pf[:], xT[:, k, :], Wkv[:, k, 0:512], k == 0, k == 7, [xTk] + wk("Wkv"), [pfk])
                        banks["k"] = (pf, pfk)
                    for g in range(3):
                        pf, pfk = nextf()
                        for k in range(8):
                            mm(pf[:], xT[:, k, :], Wq[:, k, g * 512:(g + 1) * 512], k == 0, k == 7,
                               [xTk] + wk("Wq"), [pfk])
                        banks[g] = (pf, pfk)
                    ctx[("b", t)] = banks

                def hr(pf, pfk, col, t, outv, outk):
                    tb = tbs[chain[0] % 2]; chain[0] += 1
                    headnorm_rope(tb, pf, pfk, col, t, outv, outk)

                def Sc(t):
                    banks = ctx.pop(("b", t))
                    tsl = slice(t * 128, (t + 1) * 128)
                    i2 = t % 2
                    if j == 0:
                        pfv, pfvk = banks["v"]
                        OP("act", lambda e: e.activation(
                            out=VT[:, :, tsl], in_=pfv[:].rearrange("p (c t) -> p c t", c=4), func=AF.Copy),
                           [pfvk], ["VT"])
                        pf, pfk = banks["k"]
                        hr(pf, pfk, RW["knorm"], t, kr[i2][:], ("kr", i2))
                    for g in range(3):
                        pf, pfk = banks[g]
                        hr(pf, pfk, qn, t, qr[i2][:, g * 4:(g + 1) * 4, :], ("qr", i2, g))

                def Sd(t):
                    tsl = slice(t * 128, (t + 1) * 128)
                    i2 = t % 2
                    if j == 0:
                        pb, pbk = nextb()
                        for h in range(4):
                            OP("pe", lambda e, h=h: e.transpose(out=pb[:, h * 128:(h + 1) * 128], in_=kr[i2][:, h, :],
                                                                identity=identb[:]), [("kr", i2), "identb"], [pbk])
                        OP("act", lambda e: e.activation(
                            out=KT[:, :, tsl], in_=pb[:, 0:512].rearrange("p (c t) -> p c t", c=4), func=AF.Copy),
                           [pbk], ["KT"])
                    qs = qTs[i2]
                    qsk = ("qTs", i2)
                    for half in range(2):
                        nh = 8 if half == 0 else 4
                        pb2, pb2k = nextb()
                        for hh in range(nh):
                            gh = half * 8 + hh
                            OP("pe", lambda e, hh=hh, gh=gh, pb2=pb2: e.transpose(
                                out=pb2[:, hh * 128:(hh + 1) * 128], in_=qr[i2][:, gh, :], identity=identb[:]),
                               [("qr", i2, gh // 4), "identb"], [pb2k])
                        OP("act", lambda e, pb2=pb2, half=half, nh=nh: e.activation(
                            out=qs[:, half * 8:half * 8 + nh, :],
                            in_=pb2[:, 0:nh * 128].rearrange("p (c t) -> p c t", c=nh), func=AF.Copy),
                           [pb2k], [(qsk, half)])
                    S.dma("pool", ("qst", i2),
                          [lambda e: e.dma_start(
                              out=qT_d[:, :, :, tsl].rearrange("h d g t -> d g h t"),
                              in_=qs[:].rearrange("p (g h) t -> p g h t", g=3))],
                          reads=[(qsk, 0), (qsk, 1)], writes=["qT_d"])

                for t in range(min(3, ntiles)):
                    ld(t)
                Sa(0)
                if ntiles > 1:
                    Sa(1)
                Sb(0)
                for t in range(ntiles):
                    Sc(t)
                    if t + 3 < ntiles:
                        ld(t + 3)
                    if t + 2 < ntiles:
                        Sa(t + 2)
                    if t + 1 < ntiles:
                        Sb(t + 1)
                    Sd(t)
                S.barrier()

        def phase_B2(KT, VT, oT):
            S.new_epoch()
            S.barrier()
            es = contextlib.ExitStack()
            with es:
                QTh = sb(es, "QTh", [128, 3, S_LEN], BF16)
                Vg = [sb(es, "Vg%d" % g, [128, 32, 128], BF16) for g in range(3)]
                num = sb(es, "num", [128, S_LEN], F32)
                den = sb(es, "den", [128, S_LEN], F32)
                PTa = [sb(es, "PTa%d" % i, [128, 4, 128], BF16) for i in range(4)]
                pc = [0]
                for h in range(4):
                    S.dma("sp", "qld", [lambda e, h=h: e.dma_start(out=QTh[:], in_=qT_d[h])],
                          reads=["qT_d"], writes=["QTh"])
                    for g, r in enumerate((1, 4, 16)):
                        nbk = 32 // r
                        for b0 in range(0, 32, 8):
                            pb, pbk = nextb()
                            for bb in range(8):
                                blk = b0 + bb
                                c, n = blk // nbk, blk % nbk
                                base = c + r * 128 * n
                                OP("pe", lambda e, pb=pb, bb=bb, h=h, base=base, r=r: e.transpose(
                                    out=pb[:, bb * 128:(bb + 1) * 128],
                                    in_=VT[:, h, base:base + r * 127 + 1:r], identity=identb[:]),
                                   ["VT", "identb"], [pbk])
                            OP("act", lambda e, pb=pb, g=g, b0=b0: e.activation(
                                out=Vg[g][:, b0:b0 + 8, :], in_=pb[:].rearrange("p (c t) -> p c t", c=8),
                                func=AF.Copy), [pbk], [("Vg", g)])
                    items = []
                    for g, r in enumerate((1, 4, 16)):
                        if g == 0:
                            for m in range(8):
                                items.append((g, r, [(0, 4 * m + jb) for jb in range(4)],
                                              lambda a, m=m: a[:, 512 * m:512 * (m + 1)].rearrange(
                                                  "p (j i) -> p j i", j=4)))
                        elif g == 1:
                            for n in range(8):
                                items.append((g, r, [(c, n) for c in range(4)],
                                              lambda a, n=n: a[:, 512 * n:512 * (n + 1)].rearrange(
                                                  "p (i c) -> p c i", c=4)))
                        else:
                            for n in range(2):
                                for m in range(4):
                                    items.append((g, r, [(4 * m + jb, n) for jb in range(4)],
                                                  lambda a, n=n, m=m: a[:, 2048 * n:2048 * (n + 1)].rearrange(
                                                      "p (i c) -> p c i", c=16)[:, 4 * m:4 * m + 4, :]))

                    def P(item):
                        g, r, blocks, view = item
                        pts = []
                        for half in range(2):
                            ps_, psk = nextf()
                            pt_ = PTa[pc[0] % 4]
                            ptk = ("PTa", pc[0] % 4)
                            pc[0] += 1
                            for q2 in range(2):
                                c, n = blocks[half * 2 + q2]
                                b_ = c + r * 128 * n
                                qsl = slice(b_, b_ + r * 127 + 1, r)
                                mm(ps_[:, q2 * 256:q2 * 256 + 128], KT[:, h, qsl], QTh[:, g, qsl], True, True,
                                   ["KT", "QTh"], [psk])
                                if n >= 1:
                                    bp = c + r * 128 * (n - 1)
                                    psl = slice(bp, bp + r * 127 + 1, r)
                                    mm(ps_[:, q2 * 256 + 128:q2 * 256 + 256], KT[:, h, psl], QTh[:, g, qsl],
                                       True, True, ["KT", "QTh"], [psk])
                            OP("act", lambda e, ps_=ps_, pt_=pt_: e.activation(
                                out=pt_[:].rearrange("p a b -> p (a b)"), in_=ps_[:], func=AF.Exp, scale=SCALE),
                               [psk], [ptk])
                            OP("dve", lambda e, pt_=pt_: e.tensor_tensor(out=pt_[:], in0=pt_[:], in1=mask2[:],
                                                                        op=ALU.mult), [ptk, "mask2"], [ptk])
                            pts.append((pt_, ptk))
                        return pts

                    def Q(item, pts):
                        g, r, blocks, view = item
                        nbk = 32 // r
                        pO, pOk = nextf()
                        pD, pDk = nextf()
                        for jb in range(4):
                            c, n = blocks[jb]
                            pt_, ptk = pts[jb // 2]
                            q2 = jb % 2
                            bi = c * nbk + n
                            osl = slice(jb * 128, (jb + 1) * 128)
                            mm(pO[:, osl], Vg[g][:, bi, :], pt_[:, q2 * 2, :], True, n == 0,
                               [("Vg", g), ptk], [pOk])
                            if n >= 1:
                                mm(pO[:, osl], Vg[g][:, bi - 1, :], pt_[:, q2 * 2 + 1, :], False, True,
                                   [("Vg", g), ptk], [pOk])
                            mm(pD[:, osl], onesb[:], pt_[:, q2 * 2, :], True, n == 0, ["onesb", ptk], [pDk])
                            if n >= 1:
                                mm(pD[:, osl], onesb[:], pt_[:, q2 * 2 + 1, :], False, True, ["onesb", ptk], [pDk])
                        nv = view(num)
                        dv = view(den)
                        pOv = pO[:].rearrange("p (j i) -> p j i", j=4)
                        pDv = pD[:].rearrange("p (j i) -> p j i", j=4)
                        if g == 0:
                            OP("act", lambda e: e.activation(out=nv, in_=pOv, func=AF.Copy), [pOk], ["num"])
                            OP("dve", lambda e: e.tensor_copy(out=dv, in_=pDv), [pDk], ["den"])
                        else:
                            OP("dve", lambda e: e.tensor_tensor(out=nv, in0=nv, in1=pOv, op=ALU.add),
                               [pOk, "num"], ["num"])
                            OP("dve", lambda e: e.tensor_tensor(out=dv, in0=dv, in1=pDv, op=ALU.add),
                               [pDk, "den"], ["den"])

                    cur = P(items[0])
                    for bi_ in range(len(items)):
                        nxt = P(items[bi_ + 1]) if bi_ + 1 < len(items) else None
                        Q(items[bi_], cur)
                        cur = nxt
                    OP("act", lambda e: e.activation(out=den[:], in_=den[:], func=AF.Ln), ["den"], ["den"])
                    OP("act", lambda e: e.activation(out=den[:], in_=den[:], func=AF.Exp, scale=-1.0), ["den"], ["den"])
                    OP("dve", lambda e, h=h: e.tensor_tensor(out=oT[:, h, :], in0=num[:], in1=den[:], op=ALU.mult),
                       ["num", "den"], ["oT"])
                S.barrier()

        def phase_B3(l, j, hsrc, hdst, oT):
            S.new_epoch()
            S.barrier()
            es = contextlib.ExitStack()
            with es:
                bufs = alloc_ple_w(es)
                Wz = sb(es, "Wz", [128, 8, 512], BF16)
                Wob = sb(es, "Wob", [128, 4, 1024], BF16)
                load_weights([(Wz, w_in_b_d[j][:, :, 1536:2048], 8, 512, CV[("norm_b", j)], "Wz"),
                              (Wob, w_out_b_d[j], 4, 1024, None, "Wob"),
                              (bufs["Wg"], w_pg_d[l], 8, 1024, CV[("ple_norm", l)], "Wg"),
                              (bufs["Wp"], w_pp_d[l], 2, 1024, None, "Wp")], nstg=3)
                alloc_common(es, bufs)
                Tz = sb(es, "Tz", [128, 512], F32)
                yTs = [sb(es, "yTb%d" % i, [128, 4, 128], BF16) for i in range(2)]
                ctx = {}

                def s0(t):
                    hs = t % 3
                    ctx[t] = norm_T(bufs, bufs["ht"][hs][:], ("ht", hs), slot=t % 2)

                def s1(t):
                    xT, xTk = ctx.pop(t)
                    tsl = slice(t * 128, (t + 1) * 128)
                    yT = yTs[t % 2]
                    pf, pfk = nextf()
                    for c in range(4):
                        for k in range(8):
                            mm(pf[:, c * 128:(c + 1) * 128], Wz[:, k, c * 128:(c + 1) * 128], xT[:, k, :],
                               k == 0, k == 7, [xTk] + wk("Wz"), [pfk])
                    sigmoid_inplace(Tz[:], pf[:], [pfk], "Tz")
                    OP("dve", lambda e: e.tensor_tensor(out=Tz[:], in0=pf[:], in1=Tz[:], op=ALU.mult),
                       [pfk, "Tz"], ["Tz"])
                    OP("dve", lambda e: e.tensor_tensor(
                        out=yT[:], in0=Tz[:].rearrange("p (c t) -> p c t", c=4), in1=oT[:, :, tsl], op=ALU.mult),
                       ["Tz", "oT"], [("yTb", t % 2)])

                def s3(t):
                    hs = t % 3
                    X = bufs["ht"][hs]
                    hk = ("ht", hs)
                    yT = yTs[t % 2]
                    for nb in range(2):
                        sl = slice(nb * 512, (nb + 1) * 512)
                        pf2, pf2k = nextf()
                        for c in range(4):
                            mm(pf2[:], yT[:, c, :], Wob[:, c, sl], c == 0, c == 3,
                               [("yTb", t % 2)] + wk("Wob"), [pf2k])
                        OP("dve", lambda e, sl=sl, pf2=pf2: e.tensor_tensor(out=X[:, sl], in0=X[:, sl], in1=pf2[:],
                                                                           op=ALU.add), [pf2k, hk], [hk])
                    ple_and_store(bufs, l, t, hdst)

                load_h(bufs, hsrc, 0)
                if ntiles > 1:
                    load_h(bufs, hsrc, 1)
                load_p(bufs, l, 0)
                s0(0)
                s1(0)
                if ntiles > 1:
                    s0(1)
                for t in range(ntiles):
                    if t + 2 < ntiles:
                        load_h(bufs, hsrc, t + 2)
                    if t + 1 < ntiles:
                        load_p(bufs, l, t + 1)
                        s1(t + 1)
                    if t + 2 < ntiles:
                        s0(t + 2)
                    s3(t)
                S.barrier()

        srcs = [x_d, hA_d, hB_d, hA_d]
        dsts = [hA_d, hB_d, hA_d, out_d]
        dsts[n_layers - 1] = out_d
        kvs = contextlib.ExitStack()
        KT = VT = None
        for l in range(n_layers):
            if l < 2:
                layer_A(l, srcs[l], dsts[l])
            else:
                j = l - 2
                if j == 0:
                    KT = sb(kvs, "KT", [128, 4, S_LEN], BF16)
                    VT = sb(kvs, "VT", [128, 4, S_LEN], BF16)
                    if ntiles < NT:
                        OP("pool", lambda e: e.memset(KT[:], 0.0), [], ["KT"])
                        OP("pool", lambda e: e.memset(VT[:], 0.0), [], ["VT"])
                phase_B1(l, j, srcs[l], KT, VT)
                with contextlib.ExitStack() as os_:
                    oT = sb(os_, "oT", [128, 4, S_LEN], BF16)
                    phase_B2(KT, VT, oT)
                    phase_B3(l, j, srcs[l], dsts[l], oT)
        kvs.close()
        finals = [o for k, o in S.last.items() if isinstance(k, tuple) and k[0] == "dma" and
                  isinstance(k[1], tuple) and k[1][0] == "st"]
        S.finalize_and_emit(final_waits=finals)
    return nc, S


def _wl(w):
    w = np.asarray(w, np.float32)
    return np.ascontiguousarray(w.reshape(-1, 128, w.shape[-1]).transpose(1, 0, 2))


def host_inputs(inp):
    g = {}
    g["w_in_a"] = np.stack([_wl(inp["w_in_a"][l]) for l in range(2)])
    g["w_out_a"] = np.stack([_wl(inp["w_out_a"][l]) for l in range(2)])
    g["w_kv"] = _wl(inp["w_kv"])
    g["w_in_b"] = np.stack([_wl(inp["w_in_b"][l]) for l in range(2)])
    g["w_out_b"] = np.stack([_wl(inp["w_out_b"][l]) for l in range(2)])
    g["w_ple_gate"] = np.stack([_wl(inp["w_ple_gate"][l]) for l in range(4)])
    g["w_ple"] = np.stack([_wl(inp["w_ple"][l]) for l in range(4)])
    cv = np.zeros((128, NCV), np.float32)
    for l in range(2):
        c = CV[("norm_a", l)]; cv[:, c:c + 8] = _pk(inp["norm_a"][l])
        c = CV[("hnorm_a", l)]; cv[:, c:c + 8] = _pk(inp["hnorm_a"][l])
        c = CV[("conv_a", l)]
        for j in range(4):
            cv[:, c + j * 8:c + j * 8 + 8] = _pk(inp["conv_a"][l][j])
        c = CV[("norm_b", l)]; cv[:, c:c + 8] = _pk(inp["norm_b"][l])
    c = CV["norm_kv"]; cv[:, c:c + 8] = _pk(inp["norm_kv"])
    for l in range(4):
        c = CV[("ple_norm", l)]; cv[:, c:c + 8] = _pk(inp["ple_norm"][l])
    g["cvec"] = cv
    rw = np.zeros((128, NRW), np.float32)
    for l in range(2):
        c = RW[("b_gate", l)]; rw[:, c:c + 8] = np.asarray(inp["b_gate_a"][l], np.float32)[None, :]
        c = RW[("qnorm", l)]; rw[:, c:c + 128] = np.asarray(inp["qnorm_b"][l], np.float32)[None, :]
    c = RW["knorm"]; rw[:, c:c + 128] = np.asarray(inp["knorm"], np.float32)[None, :]
    g["rows"] = rw
    inv = (np.float32(10000.0) ** (-np.arange(0, 128, 2, dtype=np.float32) / np.float32(128))).astype(np.float32)
    ang = (np.arange(S_LEN, dtype=np.float32)[:, None] * inv[None, :]).astype(np.float32)
    cos = np.cos(ang).astype(np.float32).reshape(NT, 128, 64).transpose(1, 0, 2)
    sin = np.sin(ang).astype(np.float32).reshape(NT, 128, 64).transpose(1, 0, 2)
    g["cos"] = np.ascontiguousarray(cos)
    g["sin"] = np.ascontiguousarray(sin)
    cst = np.zeros((128, 4, 128), np.float32)
    cst[:, 0, :] = np.eye(128, dtype=np.float32)
    cst[:, 1, :] = np.triu(np.ones((128, 128), np.float32))
    cst[:, 2, :] = np.tril(np.ones((128, 128), np.float32))
    cst[:, 3, :] = 1.0
    g["cst"] = cst
    return g


_CACHE = {}


def kernel(**inputs):
    inp = {k: np.asarray(v) for k, v in inputs.items()}
    B = inp["x"].shape[0]
    if "prog" not in _CACHE:
        _CACHE["prog"] = build_program(4, NT)[0]
    nc = _CACHE["prog"]
    g = host_inputs(inp)
    in_maps = []
    for b in range(B):
        m = dict(g)
        m["x"] = np.ascontiguousarray(inp["x"][b], dtype=np.float32)
        m["p"] = np.ascontiguousarray(inp["p"][:, b], dtype=np.float32)
        in_maps.append(m)
    res = run_bass_kernel_spmd(nc, in_maps, core_ids=list(range(B)))
    out = np.stack([np.asarray(r["out"], np.float32) for r in res.results], axis=0)
    return out
```
